# Optimizing a Trainium2 kernel written in Bass

```python
import math
import jax
import jax.numpy as jnp
from jax import lax
import numpy as np

D_MODEL = 1024
BATCH = 8
SEQ = 2048
DEPTH = 2

GRID_W = 64
CTX_LEN = 256

N_EVEN = (DEPTH + 1) // 2
N_ODD = DEPTH // 2

DEEPNORM_ALPHA = (2.0 * DEPTH) ** 0.25
OUT_INIT_SCALE = (8.0 * DEPTH) ** -0.25
LN_EPS = 1e-5
RMS_EPS = 1e-6

DN_HEADS = 4
DN_DK = 128
DN_DV = 128
DN_CHUNK = 64
SHORT_CONV = 4
DN_QK = DN_HEADS * DN_DK
DN_VAL = DN_HEADS * DN_DV
DN_QKV = 2 * DN_QK + DN_VAL
DN_GATES = 2 * 2 * DN_HEADS
DN_IN = DN_QKV + DN_VAL + DN_GATES

HY_WIDTH = D_MODEL // 2
HY_SHORT = 3
HY_EMB = 33
HY_FFN = 64
HY_INNER = 2
HY_TARGET = 1e-2
HY_FAST_PCT = 0.3
HY_SLOW_PCT = 1.5
HY_MIN_DECAY = math.log(HY_TARGET) / HY_SLOW_PCT
HY_MAX_DECAY = math.log(HY_TARGET) / HY_FAST_PCT
HY_IN = 3 * HY_WIDTH

EVEN_IN = DN_IN + HY_IN
EVEN_MIX = DN_VAL + HY_WIDTH

LRU_WIDTH = D_MODEL
LRU_HEADS = 4
LRU_BLOCK = LRU_WIDTH // LRU_HEADS
LRU_C = 8.0
LRU_CONV = 4
LRU_MIN_RAD = 0.9
LRU_MAX_RAD = 0.999

N_EXPERTS = 32
TOP_K = 4
D_FF = D_MODEL
SWIGLU_ALPHA = 1.702
SWIGLU_LIMIT = 7.0

kernel_name = 'hybrid_deltanet_hyena_rglru_moe_prefix'


def layer_norm(x, g, b):
    xf = x.astype(jnp.float32)
    mu = jnp.mean(xf, axis=-1, keepdims=True)
    var = jnp.mean(jnp.square(xf - mu), axis=-1, keepdims=True)
    return ((xf - mu) * lax.rsqrt(var + LN_EPS) * g + b).astype(x.dtype)


def l2norm(t):
    return t * lax.rsqrt(jnp.sum(t * t, axis=-1, keepdims=True) + RMS_EPS)


def depthwise_conv(x, w, pad_left):
    width = w.shape[0]
    length = x.shape[1]
    xp = jnp.pad(x, ((0, 0), (pad_left, width - 1 - pad_left), (0, 0)))
    out = xp[:, 0:length] * w[0]
    for i in range(1, width):
        out = out + xp[:, i:i + length] * w[i]
    return out


def sincos_2d(rows, cols, dim):
    quarter = dim // 4
    omega = 1.0 / (10000.0 ** (jnp.arange(quarter, dtype=jnp.float32) / quarter))
    def emb1d(n):
        ang = jnp.arange(n, dtype=jnp.float32)[:, None] * omega
        return jnp.concatenate([jnp.sin(ang), jnp.cos(ang)], axis=-1)
    er = jnp.broadcast_to(emb1d(rows)[:, None], (rows, cols, dim // 2))
    ec = jnp.broadcast_to(emb1d(cols)[None], (rows, cols, dim // 2))
    return jnp.concatenate([er, ec], axis=-1).reshape(rows * cols, dim)


def gated_delta_chunked(q, k, v, g, beta, s0):
    bsz, heads, length, dk = k.shape
    dv = v.shape[-1]
    csz = DN_CHUNK
    n = length // csz
    q, k, v = (t.reshape(bsz, heads, n, csz, -1) for t in (q, k, v))
    g = jnp.cumsum(g.reshape(bsz, heads, n, csz), axis=-1)
    beta = beta.reshape(bsz, heads, n, csz)
    incl = jnp.tril(jnp.ones((csz, csz), dtype=bool))
    strict = jnp.tril(jnp.ones((csz, csz), dtype=bool), -1)
    gdiff = g[..., :, None] - g[..., None, :]
    decay = jnp.where(incl, jnp.exp(jnp.where(incl, gdiff, 0.0)), 0.0)
    k_beta = k * beta[..., None]
    a_strict = jnp.where(strict, jnp.einsum('bhnid,bhnjd->bhnij', k_beta, k) * decay, 0.0)
    eye = jnp.eye(csz, dtype=jnp.float32)
    t_inv = lax.linalg.triangular_solve(eye + a_strict, jnp.broadcast_to(eye, a_strict.shape),
                                        left_side=True, lower=True, unit_diagonal=True)
    u = jnp.einsum('bhnij,bhnjd->bhnid', t_inv, v * beta[..., None])
    w = jnp.einsum('bhnij,bhnjd->bhnid', t_inv, k_beta * jnp.exp(g)[..., None])
    attn = jnp.where(incl, jnp.einsum('bhnid,bhnjd->bhnij', q, k) * decay, 0.0)
    q_dec = q * jnp.exp(g)[..., None]
    k_dec = k * jnp.exp(g[..., -1:] - g)[..., None]
    g_last = jnp.exp(g[..., -1])

    def step(state, xs):
        q_c, k_c, u_c, w_c, attn_c, gl_c = xs
        v_new = u_c - jnp.einsum('bhcd,bhde->bhce', w_c, state)
        o_c = jnp.einsum('bhcd,bhde->bhce', q_c, state) + jnp.einsum('bhij,bhje->bhie', attn_c, v_new)
        state = state * gl_c[..., None, None] + jnp.einsum('bhcd,bhce->bhde', k_c, v_new)
        return state, o_c

    xs = tuple(jnp.moveaxis(t, 2, 0) for t in (q_dec, k_dec, u, w, attn, g_last))
    s_final, o = lax.scan(step, s0, xs)
    return jnp.moveaxis(o, 0, 2).reshape(bsz, heads, length, dv), s_final


def _dn_prepare(p, conv_w, a_log, dt_bias):
    bsz, length, _ = p.shape
    qkv, z, gates = jnp.split(p, [DN_QKV, DN_QKV + DN_VAL], axis=-1)
    qkv = jax.nn.silu(depthwise_conv(qkv, conv_w, SHORT_CONV // 2).astype(jnp.float32))
    q, k, v = jnp.split(qkv, [DN_QK, 2 * DN_QK], axis=-1)
    to_heads = lambda t: t.reshape(bsz, length, DN_HEADS, -1).transpose(0, 2, 1, 3)
    q = l2norm(to_heads(q)) * (DN_DK ** -0.5)
    k = l2norm(to_heads(k))
    v = to_heads(v)
    gates = jnp.moveaxis(gates.astype(jnp.float32).reshape(bsz, length, 2, 2, DN_HEADS), 1, -1)
    dirs = []
    for d in range(2):
        g = -jnp.exp(a_log[d].astype(jnp.float32))[:, None] * jax.nn.softplus(gates[:, d, 0] + dt_bias[d][:, None])
        beta = jax.nn.sigmoid(gates[:, d, 1])
        dirs.append((g, beta))
    return q, k, v, z, dirs


def _dn_direction(q, k, v, g, beta, s0, reverse):
    if reverse:
        q, k, v = (jnp.flip(t, 2) for t in (q, k, v))
        g, beta = jnp.flip(g, -1), jnp.flip(beta, -1)
    o, s = gated_delta_chunked(q, k, v, g, beta, s0)
    if reverse:
        o = jnp.flip(o, 2)
    return o, s


def _gated_rmsnorm(o, z, gain):
    bsz, heads, length, dv = o.shape
    o = o.transpose(0, 2, 1, 3)
    o = o * lax.rsqrt(jnp.mean(o * o, axis=-1, keepdims=True) + RMS_EPS) * gain
    o = o * jax.nn.silu(z.astype(jnp.float32).reshape(bsz, length, heads, dv))
    return o.reshape(bsz, length, heads * dv)


def deltanet_mixer(p_lat, p_ctx, conv_w, a_log, dt_bias, norm_g, need_ctx):
    ql, kl, vl, zl, dirs_l = _dn_prepare(p_lat, conv_w, a_log, dt_bias)
    qc, kc, vc, zc, dirs_c = _dn_prepare(p_ctx, conv_w, a_log, dt_bias)
    s0 = jnp.zeros((p_ctx.shape[0], DN_HEADS, DN_DK, DN_DV), jnp.float32)
    o_lat = jnp.zeros_like(vl)
    o_ctx = jnp.zeros_like(vc)
    for d, reverse in enumerate((False, True)):
        oc, sc = _dn_direction(qc, kc, vc, dirs_c[d][0], dirs_c[d][1], s0, reverse)
        ol, _ = _dn_direction(ql, kl, vl, dirs_l[d][0], dirs_l[d][1], sc, reverse)
        o_lat = o_lat + ol
        if need_ctx:
            o_ctx = o_ctx + oc
    y_lat = _gated_rmsnorm(o_lat, zl, norm_g).astype(p_lat.dtype)
    y_ctx = _gated_rmsnorm(o_ctx, zc, norm_g).astype(p_ctx.dtype) if need_ctx else None
    return y_lat, y_ctx


def hyena_filter(length, w_in, b_in, w_mid, b_mid, w_out, freq):
    f32 = jnp.float32
    t = jnp.linspace(0.0, 1.0, length, dtype=f32)[:, None]
    bands = (HY_EMB - 1) // 2
    wpos = 2.0 * math.pi * jnp.arange(length, dtype=f32)[:, None] / length
    fb = jnp.linspace(1e-4, bands - 1, bands, dtype=f32)[None]
    z = jnp.concatenate([t, jnp.cos(fb * wpos), -jnp.sin(fb * wpos)], axis=-1)
    freq = freq.astype(f32)
    h = jnp.sin(freq * (z @ w_in.astype(f32) + b_in))
    for i in range(HY_INNER):
        h = jnp.sin(freq * (h @ w_mid[i].astype(f32) + b_mid[i]))
    h = (h @ w_out.astype(f32)).reshape(length, 2, HY_WIDTH)
    deltas = jnp.abs(jnp.linspace(HY_MIN_DECAY, HY_MAX_DECAY, HY_WIDTH, dtype=f32))
    h = h * jnp.exp(-t * deltas)[:, None, :]
    return h[:, 0], h[:, 1]


def centred_long_conv(u, h_fwd, h_bwd):
    length, chans = h_fwd.shape
    kern = jnp.concatenate([h_fwd, jnp.zeros((1, chans), h_fwd.dtype), h_bwd[:0:-1]], axis=0)
    uf = jnp.fft.rfft(u, n=2 * length, axis=1)
    kf = jnp.fft.rfft(kern, n=2 * length, axis=0)
    return jnp.fft.irfft(uf * kf[None], n=2 * length, axis=1)[:, :length]


def hyena_seq(p, conv_w, conv_b, filt, skip):
    length = p.shape[1]
    uc = (depthwise_conv(p, conv_w, HY_SHORT // 2) + conv_b).astype(jnp.float32)
    x0, x1, v = jnp.split(uc, 3, axis=-1)
    h_fwd, h_bwd = hyena_filter(length, *filt)
    v = v * x1
    v = centred_long_conv(v, h_fwd, h_bwd) + v * skip
    return (x0 * v).astype(p.dtype)


def even_mixer(u_lat, u_ctx, w_in, w_out, dn_conv_w, dn_a_log, dn_dt_bias, dn_norm_g,
               hy_conv_w, hy_conv_b, hy_filt, hy_skip, need_ctx):
    p_lat = u_lat @ w_in
    p_ctx = u_ctx @ w_in
    dn_lat, dn_ctx = deltanet_mixer(p_lat[..., :DN_IN], p_ctx[..., :DN_IN], dn_conv_w, dn_a_log,
                                    dn_dt_bias, dn_norm_g, need_ctx)
    hy_lat = hyena_seq(p_lat[..., DN_IN:], hy_conv_w, hy_conv_b, hy_filt, hy_skip)
    y_lat = jnp.concatenate([dn_lat, hy_lat], axis=-1) @ w_out
    y_ctx = None
    if need_ctx:
        hy_ctx = hyena_seq(p_ctx[..., DN_IN:], hy_conv_w, hy_conv_b, hy_filt, hy_skip)
        y_ctx = jnp.concatenate([dn_ctx, hy_ctx], axis=-1) @ w_out
    return y_lat, y_ctx


def rglru_coeffs(xb, wa, ba, wx, bx, a_param):
    bsz, length, width = xb.shape
    xh = xb.reshape(bsz, length, LRU_HEADS, LRU_BLOCK)
    r = jax.nn.sigmoid(jnp.einsum('blhi,hij->blhj', xh, wa.astype(jnp.float32)).reshape(bsz, length, width) + ba)
    i = jax.nn.sigmoid(jnp.einsum('blhi,hij->blhj', xh, wx.astype(jnp.float32)).reshape(bsz, length, width) + bx)
    log_a = -LRU_C * r * jax.nn.softplus(a_param.astype(jnp.float32))
    a = jnp.exp(log_a)
    b = jnp.sqrt(-jnp.expm1(2.0 * log_a)) * (i * xb)
    return a, b


def _scan_combine(left, right):
    a1, b1 = left
    a2, b2 = right
    return a1 * a2, a2 * b1 + b2


def linear_scan(a, b, h0, reverse):
    if reverse:
        a, b = jnp.flip(a, 1), jnp.flip(b, 1)
    a_cum, b_cum = lax.associative_scan(_scan_combine, (a, b), axis=1)
    h = a_cum * h0[:, None] + b_cum
    h_last = h[:, -1]
    if reverse:
        h = jnp.flip(h, 1)
    return h, h_last


def odd_mixer(u_lat, u_ctx, w_in, b_in, conv_w, conv_b, wa, ba, wx, bx, a_param, w_out, b_out, need_ctx):
    def branches(u):
        xb, yb = jnp.split(u @ w_in + b_in, 2, axis=-1)
        xb = (depthwise_conv(xb, conv_w, LRU_CONV // 2) + conv_b).astype(jnp.float32)
        return xb, yb
    x_lat, gate_lat = branches(u_lat)
    x_ctx, gate_ctx = branches(u_ctx)
    zero = jnp.zeros((u_ctx.shape[0], LRU_WIDTH), jnp.float32)
    h_lat = jnp.zeros_like(x_lat)
    h_ctx = jnp.zeros_like(x_ctx)
    for d, reverse in enumerate((False, True)):
        a_c, b_c = rglru_coeffs(x_ctx, wa[d], ba[d], wx[d], bx[d], a_param[d])
        hc, hc_last = linear_scan(a_c, b_c, zero, reverse)
        a_l, b_l = rglru_coeffs(x_lat, wa[d], ba[d], wx[d], bx[d], a_param[d])
        hl, _ = linear_scan(a_l, b_l, hc_last, reverse)
        h_lat = h_lat + hl
        if need_ctx:
            h_ctx = h_ctx + hc
    y_lat = (h_lat * jax.nn.gelu(gate_lat.astype(jnp.float32))).astype(u_lat.dtype) @ w_out + b_out
    y_ctx = None
    if need_ctx:
        y_ctx = (h_ctx * jax.nn.gelu(gate_ctx.astype(jnp.float32))).astype(u_ctx.dtype) @ w_out + b_out
    return y_lat, y_ctx


def moe_ffn(h, router_w, router_b, w1, b1, w2, b2):
    logits = (h @ router_w + router_b).astype(jnp.float32)
    top_v, top_i = lax.top_k(logits, TOP_K)
    wts = jax.nn.softmax(top_v, axis=-1)
    gate = jnp.einsum('tk,tke->te', wts, jax.nn.one_hot(top_i, N_EXPERTS, dtype=jnp.float32))
    out = jnp.zeros(h.shape, jnp.float32)
    for e in range(N_EXPERTS):
        gu = h @ w1[e] + b1[e]
        glu = jnp.minimum(gu[:, 0::2], SWIGLU_LIMIT)
        lin = jnp.clip(gu[:, 1::2], -SWIGLU_LIMIT, SWIGLU_LIMIT)
        act = glu * jax.nn.sigmoid(SWIGLU_ALPHA * glu) * (lin + 1.0)
        out = out + gate[:, e:e + 1] * (act @ w2[e] + b2[e])
    return out.astype(h.dtype)


def setup_inputs(seed: int = 0) -> dict:
    key = jax.random.key(seed)
    keys = list(jax.random.split(key, 48))
    def nrm(shape, scale):
        return jax.random.normal(keys.pop(), shape, jnp.float32) * scale
    def unif(shape, lo, hi):
        return jax.random.uniform(keys.pop(), shape, jnp.float32, lo, hi)
    d = D_MODEL
    ne, no = N_EVEN, N_ODD
    dt = jnp.exp(unif((ne, 2, DN_HEADS), math.log(1e-3), math.log(1e-1)))
    rad2 = unif((no, 2, LRU_WIDTH), LRU_MIN_RAD ** 2, LRU_MAX_RAD ** 2)
    return {
        'x': nrm((BATCH, SEQ, d), 1.0),
        'c': nrm((BATCH, d), 1.0),
        'ctx': nrm((BATCH, CTX_LEN, d), 1.0),
        'c_ctx': nrm((d,), 1.0),
        'ada_w': nrm((DEPTH, d, 6 * d), 0.5 * d ** -0.5),
        'ada_b': nrm((DEPTH, 6 * d), 0.02),
        'ln_g': 1.0 + nrm((DEPTH, 2, d), 0.02),
        'ln_b': nrm((DEPTH, 2, d), 0.02),
        'ev_w_in': nrm((ne, d, EVEN_IN), d ** -0.5),
        'ev_w_out': nrm((ne, EVEN_MIX, d), OUT_INIT_SCALE * EVEN_MIX ** -0.5),
        'dn_conv_w': nrm((ne, SHORT_CONV, DN_QKV), SHORT_CONV ** -0.5),
        'dn_a_log': jnp.log(unif((ne, 2, DN_HEADS), 1.0, 16.0)),
        'dn_dt_bias': dt + jnp.log(-jnp.expm1(-dt)),
        'dn_norm_g': 1.0 + nrm((ne, DN_DV), 0.02),
        'hy_conv_w': nrm((ne, HY_SHORT, HY_IN), HY_SHORT ** -0.5),
        'hy_conv_b': nrm((ne, HY_IN), 0.02),
        'hy_w_in': nrm((ne, HY_EMB, HY_FFN), HY_EMB ** -0.5),
        'hy_b_in': nrm((ne, HY_FFN), 0.02),
        'hy_w_mid': nrm((ne, HY_INNER, HY_FFN, HY_FFN), HY_FFN ** -0.5),
        'hy_b_mid': nrm((ne, HY_INNER, HY_FFN), 0.02),
        'hy_w_out': nrm((ne, HY_FFN, 2 * HY_WIDTH), 0.1 * HY_FFN ** -0.5),
        'hy_freq': 1.0 + nrm((ne, HY_FFN), 0.1),
        'hy_skip': nrm((ne, HY_WIDTH), 0.1),
        'od_w_in': nrm((no, d, 2 * LRU_WIDTH), d ** -0.5),
        'od_b_in': nrm((no, 2 * LRU_WIDTH), 0.02),
        'lru_conv_w': nrm((no, LRU_CONV, LRU_WIDTH), LRU_CONV ** -0.5),
        'lru_conv_b': nrm((no, LRU_WIDTH), 0.02),
        'lru_wa': nrm((no, 2, LRU_HEADS, LRU_BLOCK, LRU_BLOCK), LRU_BLOCK ** -0.5),
        'lru_ba': nrm((no, 2, LRU_WIDTH), 0.02),
        'lru_wx': nrm((no, 2, LRU_HEADS, LRU_BLOCK, LRU_BLOCK), LRU_BLOCK ** -0.5),
        'lru_bx': nrm((no, 2, LRU_WIDTH), 0.02),
        'lru_a_param': jnp.log(jnp.expm1(-0.5 * jnp.log(rad2))),
        'od_w_out': nrm((no, LRU_WIDTH, d), OUT_INIT_SCALE * LRU_WIDTH ** -0.5),
        'od_b_out': nrm((no, d), 0.02),
        'router_w': nrm((DEPTH, d, N_EXPERTS), d ** -0.5),
        'router_b': nrm((DEPTH, N_EXPERTS), 0.01),
        'moe_w1': nrm((DEPTH, N_EXPERTS, d, 2 * D_FF), d ** -0.5),
        'moe_b1': nrm((DEPTH, N_EXPERTS, 2 * D_FF), 0.02),
        'moe_w2': nrm((DEPTH, N_EXPERTS, D_FF, d), OUT_INIT_SCALE * D_FF ** -0.5),
        'moe_b2': nrm((DEPTH, N_EXPERTS, d), 0.02),
    }


def reference(x, c, ctx, c_ctx, ada_w, ada_b, ln_g, ln_b, ev_w_in, ev_w_out, dn_conv_w, dn_a_log,
              dn_dt_bias, dn_norm_g, hy_conv_w, hy_conv_b, hy_w_in, hy_b_in, hy_w_mid, hy_b_mid,
              hy_w_out, hy_freq, hy_skip, od_w_in, od_b_in, lru_conv_w, lru_conv_b, lru_wa, lru_ba,
              lru_wx, lru_bx, lru_a_param, od_w_out, od_b_out, router_w, router_b, moe_w1, moe_b1,
              moe_w2, moe_b2):
    bsz, length, dim = x.shape
    rows = length // GRID_W
    pos = sincos_2d(rows, GRID_W, dim).astype(x.dtype)
    hc = ctx
    for layer in range(DEPTH):
        last = layer == DEPTH - 1
        j = layer // 2
        mod_lat = jax.nn.silu(c) @ ada_w[layer] + ada_b[layer]
        mod_ctx = jax.nn.silu(c_ctx) @ ada_w[layer] + ada_b[layer]
        sh1, sc1, g1, sh2, sc2, g2 = jnp.split(mod_lat[:, None, :], 6, axis=-1)
        csh1, csc1, cg1, csh2, csc2, cg2 = jnp.split(mod_ctx, 6, axis=-1)
        u_lat = x * (1.0 + sc1) + sh1 + pos
        u_ctx = hc * (1.0 + csc1) + csh1
        if layer % 2 == 0:
            y_lat, y_ctx = even_mixer(u_lat, u_ctx, ev_w_in[j], ev_w_out[j], dn_conv_w[j], dn_a_log[j],
                                      dn_dt_bias[j], dn_norm_g[j], hy_conv_w[j], hy_conv_b[j],
                                      (hy_w_in[j], hy_b_in[j], hy_w_mid[j], hy_b_mid[j], hy_w_out[j], hy_freq[j]),
                                      hy_skip[j], not last)
        else:
            y_lat, y_ctx = odd_mixer(u_lat, u_ctx, od_w_in[j], od_b_in[j], lru_conv_w[j], lru_conv_b[j],
                                     lru_wa[j], lru_ba[j], lru_wx[j], lru_bx[j], lru_a_param[j],
                                     od_w_out[j], od_b_out[j], not last)
        x = layer_norm(DEEPNORM_ALPHA * x + g1 * y_lat, ln_g[layer, 0], ln_b[layer, 0])
        v_lat = (x * (1.0 + sc2) + sh2).reshape(bsz * length, dim)
        if last:
            f_lat = moe_ffn(v_lat, router_w[layer], router_b[layer], moe_w1[layer], moe_b1[layer],
                            moe_w2[layer], moe_b2[layer])
        else:
            hc = layer_norm(DEEPNORM_ALPHA * hc + cg1 * y_ctx, ln_g[layer, 0], ln_b[layer, 0])
            v_ctx = (hc * (1.0 + csc2) + csh2).reshape(-1, dim)
            f_all = moe_ffn(jnp.concatenate([v_lat, v_ctx], axis=0), router_w[layer], router_b[layer],
                            moe_w1[layer], moe_b1[layer], moe_w2[layer], moe_b2[layer])
            f_lat = f_all[:bsz * length]
            hc = layer_norm(DEEPNORM_ALPHA * hc + cg2 * f_all[bsz * length:].reshape(hc.shape),
                            ln_g[layer, 1], ln_b[layer, 1])
        x = layer_norm(DEEPNORM_ALPHA * x + g2 * f_lat.reshape(bsz, length, dim), ln_g[layer, 1], ln_b[layer, 1])
    return x
```

```python
import math
import contextlib
import numpy as np
import ml_dtypes
import concourse.bass as bass
import concourse.mybir as mybir
from concourse.bass_utils import run_bass_kernel_spmd

F32 = mybir.dt.float32
BF16 = mybir.dt.bfloat16
AF = mybir.ActivationFunctionType
ALU = mybir.AluOpType
AX = mybir.AxisListType

P = 128
D = 1024
KC = 8
TL = 2048
TC = 256
T = TL + TC
NE = 32
ALPHA = 4.0 ** 0.25
LN_EPS = 1e-5
RMS_EPS = 1e-6
N_DMA_SEMS = 40
EPOCH = 30000


def _region(ap):
    t = ap.tensor
    dims = list(ap.ap)
    tn = type(t).__name__
    if "DRam" in tn:
        lo = ap.offset
        hi = ap.offset
        for st, cnt in dims:
            if st >= 0:
                hi += st * (cnt - 1)
            else:
                lo += st * (cnt - 1)
        return (t.name, 0, 1, lo, hi + 1)
    if "PSum" in tn or "Psum" in tn or "PSUM" in tn:
        return (t.name, 0, 128, 0, 1 << 30)
    F = 1
    for s in list(t.shape)[1:]:
        F *= s
    p0 = ap.offset // F
    f0 = ap.offset % F
    pst, pcnt = dims[0]
    npart = 1 if pst == 0 else pcnt
    lo = f0
    hi = f0
    for st, cnt in dims[1:]:
        if st >= 0:
            hi += st * (cnt - 1)
        else:
            lo += st * (cnt - 1)
    return (t.name, p0, p0 + npart, lo, hi + 1)


def _overlap(a, b):
    return a[1] < b[2] and b[1] < a[2] and a[3] < b[4] and b[3] < a[4]


def _covers(a, b):
    return a[1] <= b[1] and a[2] >= b[2] and a[3] <= b[3] and a[4] >= b[4]


class _Op:
    __slots__ = ("eng", "fn", "deps", "signal", "dma", "idx", "sem", "target")

    def __init__(self, eng, fn, dma):
        self.eng = eng
        self.fn = fn
        self.deps = ()
        self.signal = False
        self.dma = dma
        self.sem = None
        self.target = None


class Sched:
    ENGS = ("pe", "dve", "act", "pool", "sp")

    def __init__(self, nc, stack):
        self.nc = nc
        self.stack = stack
        self.ops = []
        self.h = {"pe": nc.tensor, "dve": nc.vector, "act": nc.scalar, "pool": nc.gpsimd, "sp": nc.sync}
        self.rec = {}
        self.emitted = 0
        self.sems = {e: [] for e in self.ENGS}
        self.dma_sems = [stack.enter_context(nc.semaphore("sdma%d" % k)) for k in range(N_DMA_SEMS)]
        self.dma_cnt = [0] * N_DMA_SEMS
        self.dma_last = [None] * N_DMA_SEMS
        self.dma_rr = 0
        self.cnt = {e: 0 for e in self.ENGS}
        self.known = {e: {p: 0 for p in self.ENGS} for e in self.ENGS}
        self.known_dma = {e: set() for e in self.ENGS}
        self.n_wait = 0

    def _sem(self, eng, ep):
        lst = self.sems[eng]
        while len(lst) <= ep:
            lst.append(self.stack.enter_context(self.nc.semaphore("s%s%d" % (eng, len(lst)))))
        return lst[ep]

    def op(self, eng, fn, reads, writes, dma=False):
        o = _Op(eng, fn, dma)
        o.idx = len(self.ops)
        self.ops.append(o)
        deps = set()
        for ap in reads:
            r = _region(ap)
            ent = self.rec.setdefault(r[0], ([], []))
            for rr, oi in ent[0]:
                if _overlap(r, rr):
                    deps.add(oi)
            ent[1].append((r, o.idx))
        for ap in writes:
            r = _region(ap)
            ent = self.rec.setdefault(r[0], ([], []))
            for k in (0, 1):
                keep = []
                for rr, oi in ent[k]:
                    if oi != o.idx and _overlap(r, rr):
                        deps.add(oi)
                        if _covers(r, rr):
                            continue
                    keep.append((rr, oi))
                ent[k][:] = keep
            ent[0].append((r, o.idx))
        deps.discard(o.idx)
        o.deps = tuple(sorted(deps))
        return o

    def _wait_compute(self, eng, pe, tgt):
        if self.known[eng][pe] >= tgt:
            return
        self.known[eng][pe] = tgt
        ep = (tgt - 1) // EPOCH
        self.h[eng].wait_ge(self._sem(pe, ep), tgt - ep * EPOCH)
        self.n_wait += 1

    def _wait_dma(self, eng, d):
        if d in self.known_dma[eng]:
            return
        self.known_dma[eng].add(d)
        p = self.ops[d]
        self.h[eng].wait_ge(p.sem, p.target)
        self.n_wait += 1

    def flush(self, final=False):
        ops = self.ops
        new = ops[self.emitted:]
        for o in new:
            for d in o.deps:
                p = ops[d]
                if p.dma:
                    continue
                if p.eng == o.eng and o.eng == "pe":
                    continue
                p.signal = True
        if final:
            last = {}
            for o in new:
                if not o.dma:
                    last[o.eng] = o
            for o in last.values():
                o.signal = True
        for o in new:
            h = self.h[o.eng]
            need = {}
            for d in o.deps:
                p = ops[d]
                if p.dma:
                    self._wait_dma(o.eng, d)
                else:
                    if p.eng == o.eng and o.eng == "pe":
                        continue
                    if p.target is None:
                        raise RuntimeError("dep on unsignalled op")
                    if p.target > need.get(p.eng, 0):
                        need[p.eng] = p.target
            for pe, tgt in need.items():
                self._wait_compute(o.eng, pe, tgt)
            if o.dma:
                k = self.dma_rr % N_DMA_SEMS
                self.dma_rr += 1
                if self.dma_last[k] is not None:
                    self._wait_dma(o.eng, self.dma_last[k])
                self.dma_cnt[k] += 16
                o.sem = self.dma_sems[k]
                o.target = self.dma_cnt[k]
                self.dma_last[k] = o.idx
                o.fn(h).then_inc(o.sem, 16)
            else:
                ins = o.fn(h)
                if o.signal:
                    self.cnt[o.eng] += 1
                    o.target = self.cnt[o.eng]
                    ep = (o.target - 1) // EPOCH
                    ins.then_inc(self._sem(o.eng, ep), 1)
            o.fn = None
        self.emitted = len(ops)

    def barrier(self):
        self.flush(final=True)
        for e in self.ENGS:
            for pe in self.ENGS:
                if pe != e and self.cnt[pe] > 0:
                    self._wait_compute(e, pe, self.cnt[pe])
            for k in range(N_DMA_SEMS):
                if self.dma_last[k] is not None:
                    self._wait_dma(e, self.dma_last[k])
        self.rec = {}

    def dma(self, out, in_, eng="sp", **kw):
        return self.op(eng, lambda e: e.dma_start(out=out, in_=in_, **kw), [in_], [out], dma=True)

    def mm(self, out, lhsT, rhs, start=True, stop=True, **kw):
        return self.op("pe", lambda e: e.matmul(out, lhsT, rhs, start=start, stop=stop, **kw), [lhsT, rhs], [out])

    def tr(self, out, in_, ident):
        return self.op("pe", lambda e: e.transpose(out, in_, ident), [in_, ident], [out])

    def act(self, out, in_, func, bias=None, scale=None, accum_out=None):
        kw = {}
        rd = [in_]
        wr = [out]
        if bias is not None:
            kw["bias"] = bias
            if not isinstance(bias, (int, float)):
                rd.append(bias)
        if scale is not None:
            kw["scale"] = scale
            if not isinstance(scale, (int, float)):
                rd.append(scale)
        if accum_out is not None:
            kw["accum_out"] = accum_out
            wr.append(accum_out)
        return self.op("act", lambda e: e.activation(out, in_, func, **kw), rd, wr)

    def ts(self, eng, out, in0, s1, s2, op0, op1=None, accum_out=None):
        rd = [in0]
        wr = [out]
        for s in (s1, s2):
            if s is not None and not isinstance(s, (int, float)):
                rd.append(s)
        kw = {}
        if accum_out is not None:
            kw["accum_out"] = accum_out
            wr.append(accum_out)
        if op1 is None:
            return self.op(eng, lambda e: e.tensor_scalar(out, in0, s1, None, op0, **kw), rd, wr)
        return self.op(eng, lambda e: e.tensor_scalar(out, in0, s1, s2, op0, op1, **kw), rd, wr)

    def tt(self, eng, out, in0, in1, op):
        return self.op(eng, lambda e: e.tensor_tensor(out, in0, in1, op), [in0, in1], [out])

    def stt(self, out, in0, scalar, in1, op0, op1, accum_out=None):
        rd = [in0, in1]
        wr = [out]
        if not isinstance(scalar, (int, float)):
            rd.append(scalar)
        kw = {}
        if accum_out is not None:
            kw["accum_out"] = accum_out
            wr.append(accum_out)
        return self.op("dve", lambda e: e.scalar_tensor_tensor(out, in0, scalar, in1, op0, op1, **kw), rd, wr)

    def copy(self, eng, out, in_):
        if eng == "act":
            return self.op(eng, lambda e: e.copy(out, in_), [in_], [out])
        return self.op(eng, lambda e: e.tensor_copy(out, in_), [in_], [out])

    def memset(self, eng, ap, val):
        return self.op(eng, lambda e: e.memset(ap, val), [], [ap])

    def scan(self, out, d0, d1, init, op0, op1):
        rd = [d0, d1]
        if not isinstance(init, (int, float)):
            rd.append(init)
        return self.op("dve", lambda e: e.tensor_tensor_scan(out, d0, d1, init, op0, op1), rd, [out])

    def recip(self, out, in_):
        return self.op("dve", lambda e: e.reciprocal(out, in_), [in_], [out])

    def reduce(self, out, in_, axis, op):
        return self.op("dve", lambda e: e.tensor_reduce(out, in_, axis, op), [in_], [out])

    def max8(self, out, in_):
        return self.op("dve", lambda e: e.max(out, in_), [in_], [out])


class Phase:
    def __init__(self, K, name):
        self.K = K
        self.name = name
        self.st = contextlib.ExitStack()
        self.n = 0

    def sb(self, name, shape, dtype=F32):
        self.n += 1
        return self.st.enter_context(self.K.nc.sbuf_tensor("%s_%s_%d" % (self.name, name, self.n), list(shape), dtype))

    def ps(self, name, dtype=F32, cols=512):
        self.n += 1
        return self.st.enter_context(self.K.nc.psum_tensor("%s_%s_%d" % (self.name, name, self.n), [P, cols], dtype))

    def close(self):
        self.K.S.barrier()
        self.st.close()
        if not hasattr(self.K, "marks"):
            self.K.marks = []
        self.K.marks.append((self.name, sum(1 for o in self.K.S.ops if o.dma and o.eng == "sp")))


VEC_ITEMS = [("ada_b", 96), ("ln_g", 32), ("ln_b", 32), ("dn_conv_w", 48), ("hy_conv_w", 36), ("hy_conv_b", 12),
             ("hy_skip", 4), ("od_b_in", 16), ("lru_conv_w", 32), ("lru_conv_b", 8), ("lru_ba", 16), ("lru_bx", 16),
             ("lru_a", 16), ("od_b_out", 8), ("dn_norm_g", 1), ("hy_mlp", 4), ("moe_b1", 1024)]
VEC_OFF = {}
_o = 0
for _n, _c in VEC_ITEMS:
    VEC_OFF[_n] = _o
    _o += _c
NV = _o

C_ID, C_ONES = 0, 1


def C_L1(d):
    return 2 + d * 11


def C_U1(d):
    return 3 + d * 11


def C_STRICT(d):
    return 4 + d * 11


def C_INCLT(d):
    return 5 + d * 11


def C_E(d, lvl):
    return 6 + d * 11 + lvl


NCONST = 24


def _cols(v):
    v = np.asarray(v, np.float32).reshape(-1, P)
    return np.ascontiguousarray(v.T)


def make_consts():
    c = np.zeros((P, NCONST, P), np.float32)
    i = np.arange(P)[:, None]
    j = np.arange(P)[None, :]
    c[:, C_ID] = (i == j)
    c[:, C_ONES] = 1.0
    for d in range(2):
        if d == 0:
            c[:, C_L1(d)] = (i <= j)
            c[:, C_U1(d)] = (i > j)
            c[:, C_STRICT(d)] = (i > j)
            c[:, C_INCLT(d)] = (j >= i)
        else:
            c[:, C_L1(d)] = (i >= j)
            c[:, C_U1(d)] = (i < j)
            c[:, C_STRICT(d)] = (i < j)
            c[:, C_INCLT(d)] = (j <= i)
        for lvl in range(7):
            b = 1 << lvl
            same = (i // (2 * b)) == (j // (2 * b))
            ih = (i % (2 * b)) >= b
            jh = (j % (2 * b)) >= b
            if d == 0:
                c[:, C_E(d, lvl)] = same & ih & (~jh)
            else:
                c[:, C_E(d, lvl)] = same & (~ih) & jh
    return c


def make_pos():
    quarter = D // 4
    omega = (1.0 / (10000.0 ** (np.arange(quarter, dtype=np.float32) / np.float32(quarter)))).astype(np.float32)

    def emb1d(n):
        ang = (np.arange(n, dtype=np.float32)[:, None] * omega).astype(np.float32)
        return np.concatenate([np.sin(ang), np.cos(ang)], axis=-1).astype(np.float32)
    rows, colsn = TL // 64, 64
    er = np.broadcast_to(emb1d(rows)[:, None], (rows, colsn, D // 2))
    ec = np.broadcast_to(emb1d(colsn)[None], (rows, colsn, D // 2))
    pos = np.concatenate([er, ec], axis=-1).reshape(rows * colsn, D)
    return np.ascontiguousarray(pos.T).astype(np.float32)


def make_hy_z(length):
    t = np.linspace(0.0, 1.0, length, dtype=np.float32)[:, None]
    bands = 16
    wpos = (2.0 * math.pi * np.arange(length, dtype=np.float32)[:, None] / length).astype(np.float32)
    fb = np.linspace(1e-4, bands - 1, bands, dtype=np.float32)[None]
    z = np.concatenate([t, np.cos(fb * wpos), -np.sin(fb * wpos)], axis=-1).astype(np.float32)
    return np.ascontiguousarray(z.T), t[:, 0]


def make_dft(L):
    n = 2 * L
    k = (np.arange(L, dtype=np.int64)[:, None] * np.arange(L, dtype=np.int64)[None, :]) % n
    ang = 2.0 * math.pi * k.astype(np.float64) / n
    return np.cos(ang).astype(ml_dtypes.bfloat16), np.sin(ang).astype(ml_dtypes.bfloat16)


HY_MIN_DECAY = math.log(1e-2) / 1.5
HY_MAX_DECAY = math.log(1e-2) / 0.3


LAT_TILES = [(0, 512), (512, 512), (1024, 512), (1536, 512)]
ALL_TILES = [(0, 512, 0), (512, 512, 0), (1024, 512, 0), (1536, 512, 0), (2048, 256, 1)]


class KB:
    def __init__(self, nc, stack, cfg):
        self.nc = nc
        self.cfg = cfg
        self.S = Sched(nc, stack)
        self.stack = stack
        self.d = {}
        self.dump = set(cfg.get("dump", ()))
        self.outs = []

    def inp(self, name, shape, dtype=F32):
        self.d[name] = self.nc.dram_tensor(name, list(shape), dtype, kind="ExternalInput").ap()
        return self.d[name]

    def scr(self, name, shape, dtype=F32, out=False):
        if name in self.cfg.get("scr_inputs", ()):
            return self.inp(name, shape, dtype)
        kind = "ExternalOutput" if (out or name in self.dump) else "Internal"
        if kind == "ExternalOutput":
            self.outs.append(name)
        self.d[name] = self.nc.dram_tensor(name, list(shape), dtype, kind=kind).ap()
        return self.d[name]

    def vcol(self, name, i):
        o = VEC_OFF[name] + i
        return self.vec[:, o:o + 1]

    def cst(self, i):
        return self.consts[:, i, :]

    def setup(self):
        nc, S, st = self.nc, self.S, self.stack
        self.vec = st.enter_context(nc.sbuf_tensor("vec_sb", [P, NV], F32))
        self.consts = st.enter_context(nc.sbuf_tensor("consts_sb", [P, NCONST, P], F32))
        self.mod = st.enter_context(nc.sbuf_tensor("mod_sb", [P, 2, 48, 2], F32))
        self.onep = st.enter_context(nc.sbuf_tensor("onep", [P, 2, 2, 8, 2], F32))
        self.kc_ = st.enter_context(nc.sbuf_tensor("kconst", [P, 8], F32))
        S.dma(self.vec[:], self.d["vec"])
        S.dma(self.consts[:], self.d["consts"])
        vals = [1.0, LN_EPS, RMS_EPS, 0.0, -1.0, math.pi, 1e-30, 0.5]
        for i, v in enumerate(vals):
            S.memset("pool", self.kc_[:, i:i + 1], v)
        b1o = VEC_OFF["moe_b1"]
        b1v = self.vec[:, b1o:b1o + 1024].rearrange("p (a g j) -> p a g j", g=2, j=8)
        S.ts("dve", b1v[:, :, 1, :], b1v[:, :, 1, :], 1.0, None, ALU.add)
        self.c_one = self.kc_[:, 0:1]
        self.c_lneps = self.kc_[:, 1:2]
        self.c_rmseps = self.kc_[:, 2:3]
        self.c_zero = self.kc_[:, 3:4]

    def phase_mod(self):
        S = self.S
        ph = Phase(self, "mod")
        cc = ph.sb("cc", [P, 16])
        sc = ph.sb("sc", [P, 16])
        S.dma(cc[:], self.d["cc"])
        S.act(sc[:], cc[:], AF.Silu)
        ident = self.cst(C_ID)
        psA = [ph.ps("psA") for _ in range(2)]
        psB = [ph.ps("psB") for _ in range(2)]
        psT = [ph.ps("psT") for _ in range(2)]
        wb = [ph.sb("w", [P, 8, 768]) for _ in range(2)]
        rows = [ph.sb("row", [2, 768]) for _ in range(2)]
        bb = ph.sb("bb", [2, 2, 6 * D])
        for l in range(2):
            S.dma(bb[:, l, :], self.d["ada_b_raw"][l].partition_broadcast(2))
        it = 0
        for l in range(2):
            src = self.d["ada_w"][l].rearrange("(kc p) j -> p kc j", p=P)
            for jb in range(8):
                w = wb[it % 2]
                pA, pB, pT, row = psA[it % 2], psB[it % 2], psT[it % 2], rows[it % 2]
                it += 1
                S.dma(w[:], src[:, :, jb * 768:(jb + 1) * 768])
                for kc in range(8):
                    S.mm(pA[0:2, 0:512], sc[:, 2 * kc:2 * kc + 2], w[:, kc, 0:512], start=(kc == 0), stop=(kc == 7))
                for kc in range(8):
                    S.mm(pB[0:2, 0:256], sc[:, 2 * kc:2 * kc + 2], w[:, kc, 512:768], start=(kc == 0), stop=(kc == 7))
                S.tt("dve", row[:, 0:512], pA[0:2, 0:512], bb[:, l, jb * 768:jb * 768 + 512], ALU.add)
                S.tt("dve", row[:, 512:768], pB[0:2, 0:256], bb[:, l, jb * 768 + 512:(jb + 1) * 768], ALU.add)
                for jj in range(6):
                    S.tr(pT[:, 2 * jj:2 * jj + 2], row[0:2, jj * 128:(jj + 1) * 128], ident[:2, :2])
                S.copy("act", self.mod[:, l, jb * 6:(jb + 1) * 6, :], pT[:, 0:12].rearrange("p (a b) -> p a b", b=2))
            S.ts("pool", self.onep[:, l, 0, :, :], self.mod[:, l, 8:16, :], 1.0, None, ALU.add)
            S.ts("pool", self.onep[:, l, 1, :, :], self.mod[:, l, 32:40, :], 1.0, None, ALU.add)
        if "mod" in self.dump:
            o = self.scr("mod", [P, 2 * 48 * 2])
            S.dma(o, self.mod[:].rearrange("p a b c -> p (a b c)"))
        ph.close()

    def m_sh1(self, l, kc, w):
        return self.mod[:, l, 0 + kc, w:w + 1]

    def m_g1(self, l, kc, w):
        return self.mod[:, l, 16 + kc, w:w + 1]

    def m_sh2(self, l, kc, w):
        return self.mod[:, l, 24 + kc, w:w + 1]

    def m_g2(self, l, kc, w):
        return self.mod[:, l, 40 + kc, w:w + 1]

    def m_a1(self, l, kc, w):
        return self.onep[:, l, 0, kc, w:w + 1]

    def m_a2(self, l, kc, w):
        return self.onep[:, l, 1, kc, w:w + 1]

    def make_u(self, ph, l, xsrc, t0, n, w, xt, pt, u):
        S = self.S
        S.dma(xt[:, :, :n], xsrc.rearrange("(kc p) t -> p kc t", p=P)[:, :, t0:t0 + n])
        if w == 0:
            S.dma(pt[:, :, :n], self.d["posT"].rearrange("(kc p) t -> p kc t", p=P)[:, :, t0:t0 + n])
        for kc in range(8):
            if w == 0:
                S.stt(pt[:, kc, :n], xt[:, kc, :n], self.m_a1(l, kc, 0), pt[:, kc, :n], ALU.mult, ALU.add)
                S.act(u[:, kc, :n], pt[:, kc, :n], AF.Identity, bias=self.m_sh1(l, kc, 0))
            else:
                S.ts("dve", u[:, kc, :n], xt[:, kc, :n], self.m_a1(l, kc, 1), self.m_sh1(l, kc, 1), ALU.mult, ALU.add)

    def phase_l0_proj(self):
        S = self.S
        ph = Phase(self, "l0a")
        wsb = ph.sb("w", [P, 8, 3600], BF16)
        src = self.d["ev_w_in"].rearrange("(kc p) n -> p kc n", p=P)
        for q in range(4):
            S.dma(wsb[:, 2 * q:2 * q + 2, :], src[:, 2 * q:2 * q + 2, :], eng="pool")
        xts = [ph.sb("xt", [P, 8, 512]) for _ in range(2)]
        pts = [ph.sb("pt", [P, 8, 512]) for _ in range(2)]
        us = [ph.sb("u", [P, 8, 512], BF16) for _ in range(2)]
        stg = [ph.sb("stg", [P, 4, 512]) for _ in range(3)]
        pss = [ph.ps("ps") for _ in range(4)]
        pT = self.d["pT"]
        ip = 0
        ist = 0
        for ti, (t0, n, w) in enumerate(ALL_TILES):
            xt, pt, u = xts[ti % 2], pts[ti % 2], us[ti % 2]
            self.make_u(ph, 0, self.d["xT0"], t0, n, w, xt, pt, u)
            for m0 in range(0, 29, 4):
                sg = stg[ist % 3]
                ist += 1
                ms = list(range(m0, min(m0 + 4, 29)))
                for m in ms:
                    if m < 16:
                        c0, wd = m * 128, 128
                    elif m < 28:
                        c0, wd = 2064 + (m - 16) * 128, 128
                    else:
                        c0, wd = 2048, 16
                    p = pss[ip % 4]
                    ip += 1
                    for kc in range(8):
                        S.mm(p[:wd, :n], wsb[:, kc, c0:c0 + wd], u[:, kc, :n], start=(kc == 0), stop=(kc == 7))
                    if ip % 2 == 0:
                        S.copy("act", sg[:wd, m - m0, :n], p[:wd, :n])
                    else:
                        S.copy("dve", sg[:wd, m - m0, :n], p[:wd, :n])
                if ms[-1] < 28:
                    dst = pT[m0 * 128:(m0 + 4) * 128, t0:t0 + n].rearrange("(m p) t -> p m t", p=P)
                    S.dma(dst, sg[:, :, :n])
                else:
                    S.dma(pT[3584:3600, t0:t0 + n], sg[:16, 0, :n])
        ph.close()

    def conv_fm(self, out, x, wcol, taps, pad_left, segs, bias=None):
        S = self.S
        for (t0, n) in segs:
            o = out[:, t0:t0 + n]
            xi = x[:, t0:t0 + n]
            if bias is None:
                S.act(o, xi, AF.Copy, scale=wcol(pad_left))
            else:
                S.act(o, xi, AF.Identity, scale=wcol(pad_left), bias=bias)
            for i in range(taps):
                if i == pad_left:
                    continue
                s = i - pad_left
                if s < 0:
                    S.stt(out[:, t0 - s:t0 + n], x[:, t0:t0 + n + s], wcol(i), out[:, t0 - s:t0 + n], ALU.mult, ALU.add)
                else:
                    S.stt(out[:, t0:t0 + n - s], x[:, t0 + s:t0 + n], wcol(i), out[:, t0:t0 + n - s], ALU.mult, ALU.add)


    def phase_dn_prep(self):
        S = self.S
        ph = Phase(self, "dnp")
        pT = self.d["pT"]
        qkvT = self.d["qkvT"]
        segs = [(0, TL), (TL, TC)]
        ones = self.cst(C_ONES)
        xin = [ph.sb("xin", [P, T]) for _ in range(2)]
        cv = [ph.sb("cv", [P, T]) for _ in range(2)]
        sv = [ph.sb("sv", [P, T]) for _ in range(2)]
        sq = [ph.sb("sq", [P, T]) for _ in range(2)]
        rs = [ph.sb("rs", [P, 512]) for _ in range(2)]
        pss = [ph.ps("ps") for _ in range(2)]
        ip = 0
        for m in range(12):
            a, c, s_, q_ = xin[m % 2], cv[m % 2], sv[m % 2], sq[m % 2]
            S.dma(a[:], pT[m * 128:(m + 1) * 128, :])
            self.conv_fm(c, a, lambda i, m=m: self.vcol("dn_conv_w", i * 12 + m), 4, 2, segs)
            S.act(s_[:], c[:], AF.Silu)
            if m < 8:
                S.act(q_[:], s_[:], AF.Square)
                for (t0, n, w) in ALL_TILES:
                    p = pss[ip % 2]
                    r = rs[ip % 2]
                    ip += 1
                    S.mm(p[:, :n], ones, q_[:, t0:t0 + n])
                    S.act(r[:, :n], p[:, :n], AF.Sqrt, bias=self.c_rmseps)
                    S.recip(r[:, :n], r[:, :n])
                    scl = (128.0 ** -0.5) if m < 4 else 1.0
                    S.stt(s_[:, t0:t0 + n], s_[:, t0:t0 + n], scl, r[:, :n], ALU.mult, ALU.mult)
            S.dma(qkvT[m * 128:(m + 1) * 128, :], s_[:])
        gt = ph.sb("gt", [16, T])
        gp = ph.sb("gp", [16, 2])
        na = ph.sb("na", [16, 1])
        e1 = ph.sb("e1", [16, T])
        bt = ph.sb("bt", [16, T])
        S.dma(gt[:], pT[3584:3600, :])
        S.dma(gp[:], self.d["gparams"])
        S.act(na[:], gp[:, 1:2], AF.Exp)
        S.ts("dve", na[:], na[:], -1.0, None, ALU.mult)
        S.act(e1[:], gt[:], AF.Exp, bias=gp[:, 0:1])
        S.act(e1[:], e1[:], AF.Ln, bias=self.kc_[:16, 0:1])
        S.ts("dve", e1[:], e1[:], na[:, 0:1], None, ALU.mult)
        S.act(bt[:], gt[:], AF.Sigmoid)
        S.dma(self.d["gbT"][0:16, :], e1[:])
        S.dma(self.d["gbT"][16:32, :], bt[:])
        ph.close()

    def phase_dn_core(self):
        S = self.S
        ph = Phase(self, "dnc")
        F32R = mybir.dt.float32r

        def r(ap):
            return ap
        qkvT = self.d["qkvT"].rearrange("(m p) t -> p m t", p=P)
        gbT = self.d["gbT"]
        ident = self.cst(C_ID)
        ones = self.cst(C_ONES)
        NCH = T // P
        order = [[16, 17] + list(range(16)), [17, 16] + list(range(15, -1, -1))]
        step_of = [{c: s for s, c in enumerate(order[d])} for d in range(2)]
        O = ph.sb("O", [P, NCH, 4, P])
        zero_t = ph.sb("zero", [P, P])
        S.memset("pool", zero_t[:], 0.0)
        Sst = [[[ph.sb("S", [P, P]) for _ in range(2)] for h in range(4)] for d in range(2)]
        Sbf = [[[ph.sb("Sb", [P, P], BF16) for _ in range(2)] for h in range(4)] for d in range(2)]
        for d in range(2):
            for h in range(4):
                S.copy("dve", Sst[d][h][0][:], zero_t[:])
                S.copy("dve", Sbf[d][h][0][:], zero_t[:])
        qkv = [ph.sb("qkv", [P, 12, P]) for d in range(2)]
        qkr = [[ph.sb("qkr", [P, 8, P], BF16) for _ in range(2)] for d in range(2)]
        gb32 = [[ph.sb("gb32", [32, P]) for _ in range(2)] for d in range(2)]
        gtm = [[ph.sb("gtm", [P, 32]) for _ in range(2)] for d in range(2)]
        gs = [[ph.sb("gs", [P, 24]) for _ in range(2)] for d in range(2)]
        ktm = [[ph.sb("ktm", [P, 4, P]) for _ in range(2)] for d in range(2)]
        vtm = [[ph.sb("vtm", [P, 4, P]) for _ in range(2)] for d in range(2)]
        names_out = ["R", "WnT", "attnT", "Vb", "Kdec"]
        outb = [[[{n: ph.sb(n, [P, P], BF16) for n in names_out} for _ in range(2)] for h in range(4)] for d in range(2)]
        names_tmp = ["gU", "A", "Dt", "E0f", "av", "ot"]
        names_tmpb = ["E0", "E1", "Kbg", "X0", "X1", "Xt0", "Xt1", "H", "vn"]
        tmpb = [[{n: ph.sb(n, [P, P]) for n in names_tmp} for h in range(4)] for d in range(2)]
        for d in range(2):
            for h in range(4):
                for n in names_tmpb:
                    tmpb[d][h][n] = ph.sb(n, [P, P], BF16)
        for d in range(2):
            for h in range(4):
                tmpb[d][h]["Dm"] = tmpb[d][h]["gU"]
        ps_pa = [ph.ps("pa") for _ in range(1)]
        ps_rec = [ph.ps("rec") for _ in range(4)]
        ps_seq = [ph.ps("seq") for _ in range(2)]
        ps_misc = ph.ps("misc")
        cnt = {"pa": 0, "rec": 0, "seq": 0}

        def nxt(kind, lst):
            p = lst[cnt[kind] % len(lst)]
            cnt[kind] += 1
            return p

        def interleave(lists):
            n = max(len(x) for x in lists)
            for i in range(n):
                for x in lists:
                    if i < len(x):
                        x[i]()

        def prep_shared_d(s, d):
            c = order[d][s]
            par = s % 2
            qk = qkv[d]
            qr = qkr[d][par]
            S.dma(qk[:], qkvT[:, :, c * P:(c + 1) * P])
            S.dma(gb32[d][par][:], gbT[:, c * P:(c + 1) * P])
            S.copy("dve", r(qr[:]), qk[:, 0:8, :])
            S.tr(ps_misc[:, 0:32], gb32[d][par][:], ident[:32, :32])
            g_ = gtm[d][par]
            S.copy("act", g_[:], ps_misc[:, 0:32])
            gcols = g_[:, d * 8:d * 8 + 4]
            bcols = g_[:, 16 + d * 8 + 4:16 + d * 8 + 8]
            S.mm(ps_misc[:, 32:36], self.cst(C_L1(d)), gcols)
            S.mm(ps_misc[:, 36:40], ones, gcols)
            G = gs[d][par]
            S.copy("act", G[:, 0:8], ps_misc[:, 32:40])
            S.act(G[:, 8:16], G[:, 0:8], AF.Exp)
            S.tt("dve", G[:, 16:20], G[:, 4:8], G[:, 0:4], ALU.subtract)
            S.act(G[:, 16:20], G[:, 16:20], AF.Exp)
            S.tt("dve", G[:, 20:24], G[:, 8:12], bcols, ALU.mult)
            for h in range(4):
                pm = nxt("rec", ps_rec)
                S.tr(pm[:, 0:P], qk[:, 4 + h, :], ident)
                S.tr(pm[:, P:2 * P], qk[:, 8 + h, :], ident)
                S.copy("act", ktm[d][par][:, h, :], pm[:, 0:P])
                S.copy("act", vtm[d][par][:, h, :], pm[:, P:2 * P])

        def prep_stages(s, d, h):
            par = s % 2
            qr = qkr[d][par]
            g_ = gtm[d][par]
            G = gs[d][par]
            tb = tmpb[d][h]
            ob = outb[d][h][par]
            kT = qr[:, 4 + h, :]
            qT = qr[:, h, :]
            gcol = g_[:, d * 8 + h:d * 8 + h + 1]
            bcol = g_[:, 16 + d * 8 + 4 + h:16 + d * 8 + 4 + h + 1]
            st = {}
            L = []

            def s0():
                S.act(tb["gU"][:], self.cst(C_U1(d)), AF.Copy, scale=gcol)
                pa = nxt("pa", ps_pa)
                S.mm(pa[:, 0:P], self.cst(C_L1(d)), tb["gU"][:])
                S.mm(pa[:, P:2 * P], tb["gU"][:], self.cst(C_L1(d)))
                S.mm(pa[:, 2 * P:3 * P], r(kT), r(kT))
                S.mm(pa[:, 3 * P:4 * P], r(kT), r(qT))
                S.act(tb["Dm"][:], pa[:, 0:P], AF.Exp)
                S.act(tb["Dt"][:], pa[:, P:2 * P], AF.Exp)
                S.tt("pool", tb["Dm"][:], tb["Dm"][:], self.cst(C_STRICT(d)), ALU.mult)
                S.tt("pool", tb["Dt"][:], tb["Dt"][:], self.cst(C_INCLT(d)), ALU.mult)
                S.stt(tb["A"][:], pa[:, 2 * P:3 * P], bcol, tb["Dm"][:], ALU.mult, ALU.mult)
                S.tt("dve", ob["attnT"][:], pa[:, 3 * P:4 * P], tb["Dt"][:], ALU.mult)
            L.append(s0)

            def s1():
                S.tt("pool", tb["E0f"][:], tb["A"][:], self.cst(C_E(d, 0)), ALU.mult)
                pr = nxt("rec", ps_rec)
                S.tr(pr[:, 0:P], tb["E0f"][:], ident)
                S.tt("dve", r(tb["Xt0"][:]), ident, pr[:, 0:P], ALU.subtract)
                S.tt("pool", tb["X0"][:], ident, tb["E0f"][:], ALU.subtract)
                st["X"], st["Xt"] = tb["X0"], tb["Xt0"]
            L.append(s1)
            for lvl in range(1, 7):
                def sa(lvl=lvl):
                    E = tb["E%d" % (lvl % 2)]
                    S.tt("pool", r(E[:]), tb["A"][:], self.cst(C_E(d, lvl)), ALU.mult)
                    pr = nxt("rec", ps_rec)
                    S.mm(pr[:, 0:P], r(E[:]), r(st["Xt"][:]))
                    S.copy("act", r(tb["H"][:]), pr[:, 0:P])

                def sb(lvl=lvl):
                    X, Xt = st["X"], st["Xt"]
                    pr2 = nxt("rec", ps_rec)
                    S.mm(pr2[:, 0:P], r(X[:]), r(tb["H"][:]))
                    if lvl < 6:
                        S.mm(pr2[:, P:2 * P], r(tb["H"][:]), r(X[:]))
                    Xtn = ob["R"] if lvl == 6 else tb["Xt%d" % (lvl % 2)]
                    S.tt("dve", r(Xtn[:]), Xt[:], pr2[:, 0:P], ALU.subtract)
                    if lvl < 6:
                        Xn = tb["X%d" % (lvl % 2)]
                        S.tt("dve", r(Xn[:]), X[:], pr2[:, P:2 * P], ALU.subtract)
                        st["X"] = Xn
                    st["Xt"] = Xtn
                L.append(sa)
                L.append(sb)

            def sf():
                S.act(r(tb["Kbg"][:]), ktm[d][par][:, h, :], AF.Copy, scale=G[:, 20 + h:21 + h])
                pr = nxt("rec", ps_rec)
                S.mm(pr[:, 0:P], r(tb["Kbg"][:]), r(ob["R"][:]))
                S.act(r(ob["WnT"][:]), pr[:, 0:P], AF.Copy, scale=-1.0)
                S.ts("dve", r(ob["Vb"][:]), vtm[d][par][:, h, :], bcol, None, ALU.mult)
                S.act(r(ob["Kdec"][:]), ktm[d][par][:, h, :], AF.Copy, scale=G[:, 16 + h:17 + h])
            L.append(sf)
            return L

        def prep_all(s):
            for d in range(2):
                prep_shared_d(s, d)
            interleave([prep_stages(s, d, h) for h in range(4) for d in range(2)])

        def seq_stages(s, d, h):
            c = order[d][s]
            par = s % 2
            qk = qkr[d][par]
            G = gs[d][par]
            first = (step_of[d][c] < step_of[1 - d][c]) or (step_of[d][c] == step_of[1 - d][c] and d == 0)
            tb = tmpb[d][h]
            ob = outb[d][h][par]
            Sold = Sst[d][h][s % 2]
            Snew = Sst[d][h][(s + 1) % 2]
            Sob = Sbf[d][h][s % 2]
            Snb = Sbf[d][h][(s + 1) % 2]

            def a():
                p1 = nxt("seq", ps_seq)
                S.mm(p1[:, 0:P], r(ob["R"][:]), r(ob["Vb"][:]), start=True, stop=False)
                S.mm(p1[:, 0:P], ob["WnT"][:], Sob[:], start=False, stop=True)
                S.copy("act", r(tb["vn"][:]), p1[:, 0:P])

            def b_():
                p2 = nxt("seq", ps_seq)
                S.mm(p2[:, 0:P], qk[:, h, :], Sob[:])
                S.mm(p2[:, P:2 * P], r(ob["attnT"][:]), r(tb["vn"][:]))
                S.mm(p2[:, 2 * P:3 * P], r(ob["Kdec"][:]), r(tb["vn"][:]))
                S.copy("act", tb["av"][:], p2[:, P:2 * P])
                if first:
                    S.stt(O[:, c, h, :], p2[:, 0:P], G[:, 8 + h:9 + h], tb["av"][:], ALU.mult, ALU.add)
                else:
                    S.stt(tb["ot"][:], p2[:, 0:P], G[:, 8 + h:9 + h], tb["av"][:], ALU.mult, ALU.add)
                    S.tt("pool", O[:, c, h, :], O[:, c, h, :], tb["ot"][:], ALU.add)
                S.stt(Snew[:], Sold[:], G[:, 12 + h:13 + h], p2[:, 2 * P:3 * P], ALU.mult, ALU.add)
                S.copy("act", Snb[:], Snew[:])
                if "dnS" in self.dump:
                    if "dnS" not in self.d:
                        self.scr("dnS", [18, 2, 4, P, P])
                    S.dma(self.d["dnS"][s, d, h], Snew[:])
            return [a, b_]

        def seq_all(s):
            interleave([seq_stages(s, d, h) for h in range(4) for d in range(2)])

        def prep(s, d):
            raise RuntimeError("unused")

        prep_all(0)
        for s in range(NCH):
            lists = [seq_stages(s, d, h) for h in range(4) for d in range(2)]
            if s + 1 < NCH:
                for d in range(2):
                    prep_shared_d(s + 1, d)
                lists = lists + [prep_stages(s + 1, d, h) for h in range(4) for d in range(2)]
            interleave(lists)
        S.dma(self.d["dnO"], O[:].rearrange("p a b c -> p (a b c)"))
        ph.close()

    def phase_dn_post(self):
        S = self.S
        ph = Phase(self, "dnq")
        NCH = T // P
        ident = self.cst(C_ID)
        O = ph.sb("O", [P, NCH, 4, P])
        S.dma(O[:].rearrange("p a b c -> p (a b c)"), self.d["dnO"])
        ps_rec = [ph.ps("rec") for _ in range(3)]
        cnt = {"rec": 0}

        def nxt(kind, lst):
            p = lst[cnt[kind] % len(lst)]
            cnt[kind] += 1
            return p
        Of = O[:].rearrange("p a b c -> p (a b) c")
        sqt = ph.sb("sqt", [P, NCH * 4, P])
        ms = ph.sb("ms", [P, NCH * 4])
        S.tt("pool", sqt[:], Of, Of, ALU.mult)
        S.reduce(ms[:], sqt[:], AX.X, ALU.add)
        S.ts("dve", ms[:], ms[:], 1.0 / 128.0, RMS_EPS, ALU.mult, ALU.add)
        S.act(ms[:], ms[:], AF.Sqrt)
        S.recip(ms[:], ms[:])
        S.tt("dve", Of, Of, ms[:].unsqueeze(2).to_broadcast([P, NCH * 4, P]), ALU.mult)
        zt = [ph.sb("zt", [P, T]) for _ in range(2)]
        mx = [ph.sb("mx", [P, T], BF16) for _ in range(2)]
        tmpf = [ph.sb("tmpf", [P, P]) for _ in range(2)]
        k = 0
        for h in range(4):
            z_, m_ = zt[h % 2], mx[h % 2]
            S.dma(z_[:], self.d["pT"][(12 + h) * 128:(13 + h) * 128, :])
            S.act(z_[:], z_[:], AF.Silu)
            for c in range(NCH):
                pm = nxt("rec", ps_rec)
                S.tr(pm[:, 0:P], O[:, c, h, :], ident)
                tf = tmpf[k % 2]
                k += 1
                S.act(tf[:], pm[:, 0:P], AF.Copy, scale=self.vcol("dn_norm_g", 0))
                S.tt("dve", m_[:, c * P:(c + 1) * P], tf[:], z_[:, c * P:(c + 1) * P], ALU.mult)
            S.dma(self.d["mixT"][h * 128:(h + 1) * 128, :], m_[:])
        ph.close()


    def phase_hy_prep(self):
        S = self.S
        ph = Phase(self, "hyp")
        pT = self.d["pT"]
        ident = self.cst(C_ID)
        segs = [(0, TL), (TL, TC)]
        NCH = T // P
        xin = [ph.sb("xin", [P, T]) for _ in range(3)]
        cv = [ph.sb("cv", [P, T]) for _ in range(3)]
        vxtm = ph.sb("vxtm", [P, NCH, 512], BF16)
        pss = [ph.ps("ps") for _ in range(2)]
        ip = 0
        for cc in range(4):
            for j in range(3):
                m = 16 + j * 4 + cc
                S.dma(xin[j][:], pT[m * 128:(m + 1) * 128, :])
                ci = j * 4 + cc
                self.conv_fm(cv[j], xin[j], lambda i, ci=ci: self.vcol("hy_conv_w", i * 12 + ci), 3, 1, segs,
                             bias=self.vcol("hy_conv_b", ci))
            S.tt("pool", cv[2][:], cv[2][:], cv[1][:], ALU.mult)
            S.dma(self.d["hyX0"][cc * 128:(cc + 1) * 128, :], cv[0][:])
            S.dma(self.d["hyVX"][cc * 128:(cc + 1) * 128, :], cv[2][:])
            for tc in range(NCH):
                p = pss[ip % 2]
                ip += 1
                S.tr(p[:, 0:P], cv[2][:, tc * P:(tc + 1) * P], ident)
                S.copy("act", vxtm[:, tc, cc * 128:(cc + 1) * 128], p[:, 0:P])
        S.dma(self.d["hyVXtm"].rearrange("(tc p) c -> p tc c", p=P), vxtm[:])
        ph.close()

    def _sin_rr(self, out, x, tmp, n):
        S = self.S
        MAG = 12582912.0
        S.ts("dve", tmp[:, :n], x[:, :n], 1.0 / (2.0 * math.pi), MAG, ALU.mult, ALU.add)
        S.ts("dve", tmp[:, :n], tmp[:, :n], MAG, None, ALU.subtract)
        S.stt(x[:, :n], tmp[:, :n], -2.0 * math.pi, x[:, :n], ALU.mult, ALU.add)
        S.ts("dve", x[:, :n], x[:, :n], -math.pi, math.pi, ALU.max, ALU.min)
        S.act(out[:, :n], x[:, :n], AF.Sin)

    def phase_hy_main(self):
        S = self.S
        ph = Phase(self, "hym")
        NCH = T // P
        hm = VEC_OFF["hy_mlp"]
        b_in, b_m0, b_m1, freq = (self.vec[:64, hm + i:hm + i + 1] for i in range(4))
        w_in = ph.sb("w_in", [33, 64])
        w_mid = ph.sb("w_mid", [64, 2, 64])
        w_out = ph.sb("w_out", [64, 1024])
        S.dma(w_in[:], self.d["hy_w_in"])
        S.dma(w_mid[:], self.d["hy_w_mid"].rearrange("i k j -> k i j"))
        S.dma(w_out[:], self.d["hy_w_out"])
        negt = ph.sb("negt", [P, 18])
        fsc = ph.sb("fsc", [P, 18])
        delt = ph.sb("delt", [P, 512])
        altc = ph.sb("altc", [P, 1], BF16)
        altr = ph.sb("altr", [1, TL], BF16)
        S.dma(negt[:], self.d["hy_negt"])
        S.dma(fsc[:], self.d["hy_fsc"])
        S.dma(delt[:], self.d["hy_delt"])
        S.dma(altc[:], self.d["hy_altc"])
        S.dma(altr[:], self.d["hy_altr"])
        vxtm = ph.sb("vxtm", [P, NCH, 512], BF16)
        S.dma(vxtm[:], self.d["hyVXtm"].rearrange("(tc p) c -> p tc c", p=P))
        hs = ph.sb("hs", [P, NCH, 512], BF16)
        hd = ph.sb("hd", [P, NCH, 512], BF16)
        Y = ph.sb("Y", [P, NCH, 2, 512], BF16)
        YL = ph.sb("YL", [1, 2, 512], BF16)
        pss = [ph.ps("ps") for _ in range(8)]
        ip = [0]

        def nps():
            p = pss[ip[0] % 8]
            ip[0] += 1
            return p

        cfgs = [(TL, "hy_zT_lat", 0, 0, "dftC_lat", "dftS_lat"), (TC, "hy_zT_ctx", 16, TL, "dftC_ctx", "dftS_ctx")]
        pa_ = Phase(self, "hymA")
        zt = pa_.sb("zt", [33, TL])
        ha = pa_.sb("ha", [64, TL])
        hb_ = pa_.sb("hb", [64, TL])
        xa = pa_.sb("xa", [64, 512])
        xb = pa_.sb("xb", [64, 512])
        dec = [pa_.sb("dec", [P, 512]) for _ in range(2)]
        hf32 = [pa_.sb("hf32", [P, 512]) for _ in range(2)]
        hb32 = [pa_.sb("hb32", [P, 512]) for _ in range(2)]
        for (L, zname, tcb, tok0, cn, sn) in cfgs:
            S.dma(zt[:, :L], self.d[zname])
            layers = [(w_in[:, :], zt, b_in, ha), (w_mid[:, 0, :], ha, b_m0, hb_), (w_mid[:, 1, :], hb_, b_m1, ha)]
            for (wl, src, bl, dst) in layers:
                for t0 in range(0, L, 512):
                    n = min(512, L - t0)
                    p = nps()
                    S.mm(p[:64, :n], wl, src[:, t0:t0 + n])
                    S.ts("dve", xa[:, :n], p[:64, :n], bl, freq, ALU.add, ALU.mult)
                    self._sin_rr(dst[:, t0:t0 + n], xa, xb, n)
            h3 = ha
            for nc_ in range(L // P):
                k = nc_ % 2
                pf = nps()
                pb = nps()
                S.mm(pf[:, :], h3[:, nc_ * P:(nc_ + 1) * P], w_out[:, 0:512])
                S.mm(pb[:, :], h3[:, nc_ * P:(nc_ + 1) * P], w_out[:, 512:1024])
                S.act(dec[k][:], delt[:], AF.Exp, scale=negt[:, tcb + nc_:tcb + nc_ + 1])
                S.tt("dve", hf32[k][:], pf[:, :], dec[k][:], ALU.mult)
                S.tt("dve", hb32[k][:], pb[:, :], dec[k][:], ALU.mult)
                if nc_ == 0:
                    S.memset("pool", hb32[k][0:1, :], 0.0)
                S.tt("pool", hs[:, tcb + nc_, :], hf32[k][:], hb32[k][:], ALU.add)
                S.tt("pool", hd[:, tcb + nc_, :], hf32[k][:], hb32[k][:], ALU.subtract)
        if "hyH" in self.dump:
            o = self.scr("hyH", [P, NCH * 512], BF16)
            S.dma(o, hs[:].rearrange("p a b -> p (a b)"))
        pa_.close()
        pb_ = Phase(self, "hymB")
        Ct = [pb_.sb("Ct", [P, 16, P], BF16) for _ in range(2)]
        St = [pb_.sb("St", [P, 16, P], BF16) for _ in range(2)]
        b1s = [pb_.sb("b1s", [P, 512]) for _ in range(2)]
        b2s = [pb_.sb("b2s", [P, 512]) for _ in range(2)]
        m1 = [pb_.sb("m1", [P, 512]) for _ in range(2)]
        m2 = [pb_.sb("m2", [P, 512]) for _ in range(2)]
        it = 0
        for (L, zname, tcb, tok0, cn, sn) in cfgs:
            ntc = L // P
            Cd = self.d[cn].rearrange("(tc p) f -> p tc f", p=P)
            Sd = self.d[sn].rearrange("(tc p) f -> p tc f", p=P)
            for fc in range(ntc):
                k = it % 2
                it += 1
                S.dma(Ct[k][:, :ntc, :], Cd[:, :, fc * P:(fc + 1) * P])
                S.dma(St[k][:, :ntc, :], Sd[:, :, fc * P:(fc + 1) * P])
                pA1, pA2, pB1, pB2 = nps(), nps(), nps(), nps()
                for (pp, tab, src) in ((pA1, Ct[k], vxtm), (pA2, St[k], vxtm), (pB1, Ct[k], hs), (pB2, St[k], hd)):
                    for tc in range(ntc):
                        S.mm(pp[:, :], tab[:, tc, :], src[:, tcb + tc, :], start=(tc == 0), stop=(tc == ntc - 1))
                S.copy("act", b1s[k][:], pB1[:, :])
                S.copy("act", b2s[k][:], pB2[:, :])
                fcol = fsc[:, tcb + fc:tcb + fc + 1]
                S.tt("dve", m1[k][:], pA1[:, :], b1s[k][:], ALU.mult)
                S.tt("dve", m2[k][:], pA2[:, :], b2s[k][:], ALU.mult)
                S.tt("pool", m1[k][:], m1[k][:], m2[k][:], ALU.subtract)
                S.act(Y[:, tcb + fc, 0, :], m1[k][:], AF.Copy, scale=fcol)
                S.tt("dve", b2s[k][:], pA1[:, :], b2s[k][:], ALU.mult)
                S.tt("dve", b1s[k][:], pA2[:, :], b1s[k][:], ALU.mult)
                S.tt("pool", b1s[k][:], b1s[k][:], b2s[k][:], ALU.add)
                S.act(Y[:, tcb + fc, 1, :], b1s[k][:], AF.Copy, scale=fcol)
            li = 0 if L == TL else 1
            pU, pK = nps(), nps()
            for tc in range(ntc):
                S.mm(pU[0:1, :], altc[:, 0:1], vxtm[:, tcb + tc, :], start=(tc == 0), stop=(tc == ntc - 1))
            for tc in range(ntc):
                S.mm(pK[0:1, :], altc[:, 0:1], hs[:, tcb + tc, :], start=(tc == 0), stop=(tc == ntc - 1))
            S.copy("act", m1[0][0:1, :], pK[0:1, :])
            S.stt(YL[0:1, li, :], pU[0:1, :], 1.0 / (2.0 * L), m1[0][0:1, :], ALU.mult, ALU.mult)
        pb_.close()
        pc_ = Phase(self, "hymC")
        Cf = [pc_.sb("Cf", [P, 16, 512], BF16) for _ in range(1)]
        Sf = [pc_.sb("Sf", [P, 16, 512], BF16) for _ in range(1)]
        x0t = [pc_.sb("x0t", [P, 512]) for _ in range(2)]
        vxt = [pc_.sb("vxt", [P, 512]) for _ in range(2)]
        ot = [pc_.sb("ot", [P, 512], BF16) for _ in range(2)]
        it = 0
        for (L, zname, tcb, tok0, cn, sn) in cfgs:
            nfc = L // P
            li = 0 if L == TL else 1
            Cd = self.d[cn].rearrange("(fc p) t -> p fc t", p=P)
            Sd = self.d[sn].rearrange("(fc p) t -> p fc t", p=P)
            for t0 in range(0, L, 512):
                n = min(512, L - t0)
                S.dma(Cf[0][:, :nfc, :n], Cd[:, :, t0:t0 + n])
                S.dma(Sf[0][:, :nfc, :n], Sd[:, :, t0:t0 + n])
                for cc in range(4):
                    k = it % 2
                    it += 1
                    S.dma(x0t[k][:, :n], self.d["hyX0"][cc * 128:(cc + 1) * 128, tok0 + t0:tok0 + t0 + n])
                    S.dma(vxt[k][:, :n], self.d["hyVX"][cc * 128:(cc + 1) * 128, tok0 + t0:tok0 + t0 + n])
                    p = nps()
                    for fc in range(nfc):
                        S.mm(p[:, :n], Y[:, tcb + fc, 0, cc * 128:(cc + 1) * 128], Cf[0][:, fc, :n], start=(fc == 0), stop=False)
                        S.mm(p[:, :n], Y[:, tcb + fc, 1, cc * 128:(cc + 1) * 128], Sf[0][:, fc, :n], start=False, stop=False)
                    S.mm(p[:, :n], YL[0:1, li, cc * 128:(cc + 1) * 128], altr[0:1, t0:t0 + n], start=False, stop=True)
                    S.stt(vxt[k][:, :n], vxt[k][:, :n], self.vcol("hy_skip", cc), p[:, :n], ALU.mult, ALU.add)
                    S.tt("pool", ot[k][:, :n], vxt[k][:, :n], x0t[k][:, :n], ALU.mult)
                    S.dma(self.d["mixT"][512 + cc * 128:512 + (cc + 1) * 128, tok0 + t0:tok0 + t0 + n], ot[k][:, :n])
        pc_.close()
        ph.close()


    def phase_post(self, l):
        S = self.S
        ph = Phase(self, "post%d" % l)
        ident = self.cst(C_ID)
        ones = self.cst(C_ONES)
        xsrc = self.d["xT0"] if l == 0 else self.d["xT1"]
        wsrc = self.d["ev_w_out"] if l == 0 else self.d["od_w_out"]
        wsb = ph.sb("w", [P, 8, D], BF16)
        S.dma(wsb[:], wsrc.rearrange("(kc p) n -> p kc n", p=P), eng="pool")
        rw = ph.sb("rw", [P, 8, NE])
        S.dma(rw[:], self.d["router_w"][l].rearrange("(kc p) e -> p kc e", p=P))
        rb = ph.sb("rb", [P, NE])
        S.dma(rb[:], self.d["router_b"][l].partition_broadcast(P))
        gbc = ph.sb("gbc", [P, 8, 2])
        for kc in range(8):
            for w in range(2):
                if l == 1:
                    S.tt("pool", gbc[:, kc, w:w + 1], self.m_g1(l, kc, w), self.vcol("od_b_out", kc), ALU.mult)
                else:
                    S.memset("pool", gbc[:, kc, w:w + 1], 0.0)
        mixs = [ph.sb("mix", [P, 8, 512], BF16) for _ in range(2)]
        xts = [ph.sb("xt", [P, 8, 512]) for _ in range(2)]
        zs = [ph.sb("z", [P, 8, 512]) for _ in range(2)]
        zsq = [ph.sb("zsq", [P, 512]) for _ in range(2)]
        x1s = [ph.sb("x1", [P, 8, 512]) for _ in range(2)]
        v32s = [ph.sb("v32", [P, 8, 512]) for _ in range(2)]
        vbs = [ph.sb("vb", [P, 8, 512], BF16)] * 2
        means = [ph.sb("mean", [P, 512])] * 2
        rstds = [ph.sb("rstd", [P, 512])] * 2
        lg = [ph.sb("lg", [P, NE]) for _ in range(2)]
        ex = [ph.sb("ex", [P, NE]) for _ in range(2)]
        mx = [ph.sb("mx", [P, 8]) for _ in range(2)]
        sm = [ph.sb("sm", [P, 4]) for _ in range(2)]
        gTs = [ph.sb("gT", [NE, 512]) for _ in range(2)]
        ps_y = [ph.ps("psy") for _ in range(2)]
        ps_s1s = [ph.ps("s1") for _ in range(2)]
        ps_s2s = [ph.ps("s2") for _ in range(2)]
        ps_lg = ph.ps("lg")
        ps_tr = ph.ps("tr")
        tiles = ALL_TILES if l == 0 else ALL_TILES[:4]
        cnt = {"iy": 0, "ir": 0}

        def stage_a(ti):
            t0, n, w = tiles[ti]
            mix, xt, z, x1, v32, vb = mixs[ti % 2], xts[ti % 2], zs[ti % 2], x1s[ti % 2], v32s[ti % 2], vbs[ti % 2]
            ps_s1, ps_s2 = ps_s1s[ti % 2], ps_s2s[ti % 2]
            S.dma(mix[:, :, :n], self.d["mixT"].rearrange("(kc p) t -> p kc t", p=P)[:, :, t0:t0 + n])
            S.dma(xt[:, :, :n], xsrc.rearrange("(kc p) t -> p kc t", p=P)[:, :, t0:t0 + n])
            for dc in range(8):
                p = ps_y[cnt["iy"] % 2]
                cnt["iy"] += 1
                for kc in range(8):
                    S.mm(p[:, :n], wsb[:, kc, dc * 128:(dc + 1) * 128], mix[:, kc, :n], start=(kc == 0), stop=(kc == 7))
                S.act(xt[:, dc, :n], xt[:, dc, :n], AF.Identity, scale=ALPHA, bias=gbc[:, dc, w:w + 1])
                S.stt(z[:, dc, :n], p[:, :n], self.m_g1(l, dc, w), xt[:, dc, :n], ALU.mult, ALU.add)
                q = zsq[dc % 2]
                S.act(q[:, :n], z[:, dc, :n], AF.Square)
                S.mm(ps_s1[:, :n], ones, z[:, dc, :n], start=(dc == 0), stop=(dc == 7))
                S.mm(ps_s2[:, :n], ones, q[:, :n], start=(dc == 0), stop=(dc == 7))
            self._ln_apply(l, 0, w, n, z, ps_s1, ps_s2, means[ti % 2], rstds[ti % 2], x1)
            S.dma(self.d["x1T"].rearrange("(kc p) t -> p kc t", p=P)[:, :, t0:t0 + n], x1[:, :, :n])
            for dc in range(8):
                S.ts("dve", v32[:, dc, :n], x1[:, dc, :n], self.m_a2(l, dc, w), self.m_sh2(l, dc, w), ALU.mult, ALU.add)
                S.copy("act", vb[:, dc, :n], v32[:, dc, :n])
            S.dma(self.d["hT"].rearrange("(kc p) t -> p kc t", p=P)[:, :, t0:t0 + n], vb[:, :, :n])

        def stage_b(ti):
            t0, n, w = tiles[ti]
            v32, gT = v32s[ti % 2], gTs[ti % 2]
            for c0 in range(0, n, P):
                k = cnt["ir"] % 2
                cnt["ir"] += 1
                for kc in range(8):
                    S.mm(ps_lg[:, 0:NE], v32[:, kc, c0:c0 + P], rw[:, kc, :], start=(kc == 0), stop=(kc == 7))
                S.tt("dve", lg[k][:], ps_lg[:, 0:NE], rb[:], ALU.add)
                S.max8(mx[k][:], lg[k][:])
                S.ts("dve", sm[k][:, 0:1], mx[k][:, 0:1], -1.0, None, ALU.mult)
                S.act(ex[k][:], lg[k][:], AF.Exp, bias=sm[k][:, 0:1])
                S.ts("dve", lg[k][:], lg[k][:], mx[k][:, 3:4], None, ALU.is_ge)
                S.tt("dve", ex[k][:], ex[k][:], lg[k][:], ALU.mult)
                S.reduce(sm[k][:, 1:2], ex[k][:], AX.X, ALU.add)
                S.recip(sm[k][:, 2:3], sm[k][:, 1:2])
                S.ts("dve", ex[k][:], ex[k][:], sm[k][:, 2:3], None, ALU.mult)
                S.tr(ps_tr[:NE, 0:P], ex[k][:], ident)
                S.copy("act", gT[:, c0:c0 + P], ps_tr[:NE, 0:P])
            S.dma(self.d["gateT"][:, t0:t0 + n], gT[:, :n])

        stage_a(0)
        for ti in range(len(tiles)):
            if ti + 1 < len(tiles):
                stage_a(ti + 1)
            stage_b(ti)
        ph.close()

    def _ln_apply(self, l, which, w, n, z, ps_s1, ps_s2, mean, rstd, out):
        S = self.S
        S.act(mean[:, :n], ps_s1[:, :n], AF.Copy, scale=1.0 / D)
        S.tt("pool", rstd[:, :n], mean[:, :n], mean[:, :n], ALU.mult)
        S.stt(rstd[:, :n], ps_s2[:, :n], 1.0 / D, rstd[:, :n], ALU.mult, ALU.subtract)
        S.ts("dve", rstd[:, :n], rstd[:, :n], LN_EPS, None, ALU.add)
        S.act(rstd[:, :n], rstd[:, :n], AF.Sqrt)
        S.recip(rstd[:, :n], rstd[:, :n])
        for dc in range(8):
            e = "pool" if dc % 2 else "dve"
            S.tt(e, z[:, dc, :n], z[:, dc, :n], mean[:, :n], ALU.subtract)
            S.tt(e, z[:, dc, :n], z[:, dc, :n], rstd[:, :n], ALU.mult)
            S.act(out[:, dc, :n], z[:, dc, :n], AF.Identity, scale=self.vcol("ln_g", (l * 2 + which) * 8 + dc),
                  bias=self.vcol("ln_b", (l * 2 + which) * 8 + dc))


    def phase_moe(self, l):
        S = self.S
        ph = Phase(self, "moe%d" % l)
        ones = self.cst(C_ONES)
        last = (l == 1)
        if not last:
            ttiles = [(0, [384, 384], [0, 0]), (768, [384, 384], [0, 0]), (1536, [384, 128, 256], [0, 0, 1])]
        else:
            ttiles = [(0, [384, 384], [0, 0]), (768, [384, 384], [0, 0]), (1536, [256, 256], [0, 0])]
        TT = 768
        xdst = self.d["out"] if last else self.d["xT1"]
        w1src = self.d["moe_w1r"]
        w2src = self.d["moe_w2r"]
        b2sb = ph.sb("b2", [NE, D])
        S.dma(b2sb[:], self.d["moe_b2"][l])
        hT = ph.sb("hT", [P, 8, TT], BF16)
        gT = ph.sb("gT", [NE, TT])
        acts = [ph.sb("act", [P, 8, TT], BF16) for _ in range(2)]
        yacc = ph.sb("yacc", [P, 8, TT])
        w1b = [ph.sb("w1", [P, 8, 1024], BF16) for _ in range(3)]
        w2b = [ph.sb("w2", [P, 8, 1024], BF16) for _ in range(2)]
        gb = [ph.sb("gb", [P, TT]) for _ in range(2)]
        NTMP = 5
        tg = [ph.sb("tg", [P, 384]) for _ in range(NTMP)]
        tsg = [ph.sb("ts", [P, 384]) for _ in range(NTMP)]
        tl = [ph.sb("tl", [P, 384]) for _ in range(NTMP)]
        mean, rstd = tg[0], tg[1]
        x1r = [tsg[0], tsg[1]]
        zsq = [tl[0], tl[1]]
        pipe = []

        def pipe_step(new):
            if new is not None:
                pipe.append(new)
                it = new
                S.ts("dve", it["g"], it["pg"], it["bg"], 7.0, ALU.add, ALU.min)
                S.act(it["l"], it["pl"], AF.Identity, bias=it["bl"])
            else:
                pipe.append(None)
            if len(pipe) >= 2 and pipe[-2] is not None:
                it = pipe[-2]
                S.act(it["s"], it["g"], AF.Sigmoid, scale=1.702)
                S.ts("dve", it["l"], it["l"], 8.0, -6.0, ALU.min, ALU.max)
                S.tt("pool", it["g"], it["g"], it["gate"], ALU.mult)
            if len(pipe) >= 3 and pipe[-3] is not None:
                it = pipe[-3]
                S.tt("pool", it["l"], it["l"], it["g"], ALU.mult)
            if len(pipe) >= 4 and pipe[-4] is not None:
                it = pipe[-4]
                S.tt("dve", it["out"], it["l"], it["s"], ALU.mult)
            if len(pipe) > 4:
                pipe.pop(0)

        def pipe_flush():
            for _ in range(3):
                pipe_step(None)
            del pipe[:]
        ps_g = [ph.ps("psg") for _ in range(2)]
        ps_l = [ph.ps("psl") for _ in range(2)]
        ps_y = [ph.ps("psy") for _ in range(2)]
        ps_s1 = ph.ps("s1")
        ps_s2 = ph.ps("s2")
        cnt = {"w1": 0, "e": 0, "gl": 0, "y": 0, "tmp": 0}
        b1o = VEC_OFF["moe_b1"]

        NEr = self.cfg.get("moe_experts", NE)
        for (tt0, subs, ws) in ttiles:
            nt = sum(subs)
            offs = [sum(subs[:i]) for i in range(len(subs))]
            S.dma(hT[:, :, :nt], self.d["hT"].rearrange("(kc p) t -> p kc t", p=P)[:, :, tt0:tt0 + nt])
            S.dma(gT[:, :nt], self.d["gateT"][:, tt0:tt0 + nt])
            for dc in range(8):
                for (o, n) in zip(offs, subs):
                    p = ps_y[cnt["y"] % 2]
                    cnt["y"] += 1
                    S.mm(p[:, :n], b2sb[:, dc * 128:(dc + 1) * 128], gT[:, o:o + n])
                    S.copy("act", yacc[:, dc, o:o + n], p[:, :n])

            wslot = {}

            def dma_w1(e, jh):
                if e >= NEr:
                    return
                wt = w1b[(2 * e + jh) % 3]
                S.dma(wt[:], w1src[l, e, jh], eng="pool")
                wslot[(e, jh)] = wt

            def dma_w2(e):
                if e >= NEr:
                    return
                S.dma(w2b[e % 2][:], w2src[l, e], eng="pool")

            def stage1(e):
                k = e % 2
                act = acts[k]
                g_b = gb[k]
                S.dma(g_b[:, :nt], self.d["gateT"][e:e + 1, tt0:tt0 + nt].partition_broadcast(P))
                dma_w1(e + 1, 0)
                dma_w2(e)
                wh = [wslot[(e, 0)], wslot[(e, 1)]]
                for j in range(8):
                    if j == 5:
                        dma_w1(e + 1, 1)
                    wt = wh[j // 4]
                    c0 = (j % 4) * 256
                    bg = self.vec[:, b1o + (l * 32 + e) * 16 + j:b1o + (l * 32 + e) * 16 + j + 1]
                    bl = self.vec[:, b1o + (l * 32 + e) * 16 + 8 + j:b1o + (l * 32 + e) * 16 + 8 + j + 1]
                    for (o, n) in zip(offs, subs):
                        pg = ps_g[cnt["gl"] % 2]
                        pl = ps_l[cnt["gl"] % 2]
                        cnt["gl"] += 1
                        for kc in range(8):
                            S.mm(pg[:, :n], wt[:, kc, c0:c0 + 128], hT[:, kc, o:o + n], start=(kc == 0), stop=(kc == 7))
                        for kc in range(8):
                            S.mm(pl[:, :n], wt[:, kc, c0 + 128:c0 + 256], hT[:, kc, o:o + n], start=(kc == 0), stop=(kc == 7))
                        q = cnt["tmp"] % NTMP
                        cnt["tmp"] += 1
                        pipe_step({"g": tg[q][:, :n], "s": tsg[q][:, :n], "l": tl[q][:, :n], "pg": pg[:, :n], "pl": pl[:, :n],
                                   "bg": bg, "bl": bl, "gate": g_b[:, o:o + n], "out": act[:, j, o:o + n]})

            def stage2(e):
                k = e % 2
                act = acts[k]
                wt = w2b[k]
                for dc in range(8):
                    for (o, n) in zip(offs, subs):
                        p = ps_y[cnt["y"] % 2]
                        cnt["y"] += 1
                        for fc in range(8):
                            S.mm(p[:, :n], wt[:, fc, dc * 128:(dc + 1) * 128], act[:, fc, o:o + n], start=(fc == 0), stop=(fc == 7))
                        S.tt("dve", yacc[:, dc, o:o + n], yacc[:, dc, o:o + n], p[:, :n], ALU.add)

            dma_w1(0, 0)
            dma_w1(0, 1)
            stage1(0)
            for e in range(NEr):
                if e + 1 < NEr:
                    stage1(e + 1)
                else:
                    pipe_flush()
                stage2(e)
            if "fT" in self.dump:
                if "fT" not in self.d:
                    self.scr("fT", [D, T])
                S.dma(self.d["fT"].rearrange("(kc p) t -> p kc t", p=P)[:, :, tt0:tt0 + nt], yacc[:, :, :nt])
            x1v = self.d["x1T"].rearrange("(kc p) t -> p kc t", p=P)
            xdv = xdst.rearrange("(kc p) t -> p kc t", p=P)
            for (o, n, w) in zip(offs, subs, ws):
                for dc in range(8):
                    xr = x1r[dc % 2]
                    S.dma(xr[:, :n], x1v[:, dc, tt0 + o:tt0 + o + n])
                    S.act(xr[:, :n], xr[:, :n], AF.Copy, scale=ALPHA)
                    S.stt(yacc[:, dc, o:o + n], yacc[:, dc, o:o + n], self.m_g2(l, dc, w), xr[:, :n], ALU.mult, ALU.add)
                    q = zsq[dc % 2]
                    S.act(q[:, :n], yacc[:, dc, o:o + n], AF.Square)
                    S.mm(ps_s1[:, :n], ones, yacc[:, dc, o:o + n], start=(dc == 0), stop=(dc == 7))
                    S.mm(ps_s2[:, :n], ones, q[:, :n], start=(dc == 0), stop=(dc == 7))
                zv = yacc[:, :, o:o + n]
                self._ln_apply2(l, 1, n, yacc, o, ps_s1, ps_s2, mean, rstd)
                S.dma(xdv[:, :, tt0 + o:tt0 + o + n], yacc[:, :, o:o + n])
        ph.close()

    def _ln_apply2(self, l, which, n, z, o, ps_s1, ps_s2, mean, rstd):
        S = self.S
        S.act(mean[:, :n], ps_s1[:, :n], AF.Copy, scale=1.0 / D)
        S.tt("pool", rstd[:, :n], mean[:, :n], mean[:, :n], ALU.mult)
        S.stt(rstd[:, :n], ps_s2[:, :n], 1.0 / D, rstd[:, :n], ALU.mult, ALU.subtract)
        S.ts("dve", rstd[:, :n], rstd[:, :n], LN_EPS, None, ALU.add)
        S.act(rstd[:, :n], rstd[:, :n], AF.Sqrt)
        S.recip(rstd[:, :n], rstd[:, :n])
        for dc in range(8):
            e = "pool" if dc % 2 else "dve"
            zz = z[:, dc, o:o + n]
            S.tt(e, zz, zz, mean[:, :n], ALU.subtract)
            S.tt(e, zz, zz, rstd[:, :n], ALU.mult)
            S.act(zz, zz, AF.Identity, scale=self.vcol("ln_g", (l * 2 + which) * 8 + dc),
                  bias=self.vcol("ln_b", (l * 2 + which) * 8 + dc))


    def phase_l1_proj(self):
        S = self.S
        ph = Phase(self, "l1a")
        wsb = ph.sb("w", [P, 8, 2048], BF16)
        src = self.d["od_w_in"].rearrange("(kc p) n -> p kc n", p=P)
        for q in range(2):
            S.dma(wsb[:, 4 * q:4 * q + 4, :], src[:, 4 * q:4 * q + 4, :], eng="pool")
        xts = [ph.sb("xt", [P, 8, 512]) for _ in range(2)]
        pts = [ph.sb("pt", [P, 8, 512]) for _ in range(2)]
        us = [ph.sb("u", [P, 8, 512], BF16) for _ in range(2)]
        stg = [ph.sb("stg", [P, 4, 512]) for _ in range(3)]
        pss = [ph.ps("ps") for _ in range(4)]
        ip = 0
        ist = 0
        for ti, (t0, n, w) in enumerate(ALL_TILES):
            xt, pt, u = xts[ti % 2], pts[ti % 2], us[ti % 2]
            self.make_u(ph, 1, self.d["xT1"], t0, n, w, xt, pt, u)
            for m0 in range(0, 16, 4):
                sg = stg[ist % 3]
                ist += 1
                for m in range(m0, m0 + 4):
                    p = pss[ip % 4]
                    ip += 1
                    for kc in range(8):
                        S.mm(p[:, :n], wsb[:, kc, m * 128:(m + 1) * 128], u[:, kc, :n], start=(kc == 0), stop=(kc == 7))
                    if m < 8:
                        S.ts("dve", sg[:, m - m0, :n], p[:, :n], self.vcol("od_b_in", m), None, ALU.add)
                    else:
                        S.act(sg[:, m - m0, :n], p[:, :n], AF.Gelu, bias=self.vcol("od_b_in", m))
                dst = self.d["lruP"][m0 * 128:(m0 + 4) * 128, t0:t0 + n].rearrange("(m p) t -> p m t", p=P)
                S.dma(dst, sg[:, :, :n])
        ph.close()

    def phase_lru(self):
        S = self.S
        ph = Phase(self, "lru")
        segs = [(0, TL), (TL, TC)]
        lruP = self.d["lruP"]
        wa = ph.sb("wa", [P, 8, 2, 256], BF16)
        wx = ph.sb("wx", [P, 8, 2, 256], BF16)
        S.dma(wa[:], self.d["lru_wa"].rearrange("d h (ic p) j -> p (d h) ic j", p=P), eng="pool")
        S.dma(wx[:], self.d["lru_wx"].rearrange("d h (ic p) j -> p (d h) ic j", p=P), eng="pool")
        nsp = ph.sb("nsp", [P, 16])
        ao = VEC_OFF["lru_a"]
        S.act(nsp[:], self.vec[:, ao:ao + 16], AF.Exp)
        S.act(nsp[:], nsp[:], AF.Ln, bias=self.c_one)
        S.ts("dve", nsp[:], nsp[:], -8.0, None, ALU.mult)
        xcb = ph.sb("xcb", [P, 8, T], BF16)
        xin = [ph.sb("xin", [P, T]) for _ in range(2)]
        xc = [ph.sb("xc", [P, T]) for _ in range(2)]
        for c in range(8):
            a, o = xin[c % 2], xc[c % 2]
            S.dma(a[:], lruP[c * 128:(c + 1) * 128, :])
            self.conv_fm(o, a, lambda i, c=c: self.vcol("lru_conv_w", i * 8 + c), 4, 2, segs, bias=self.vcol("lru_conv_b", c))
            S.dma(self.d["lruXC"][c * 128:(c + 1) * 128, :], o[:])
            S.copy("act", xcb[:, c, :], o[:])
        r_ = ph.sb("r", [P, T])
        i_ = ph.sb("i", [P, T])
        a_ = ph.sb("a", [P, T])
        b_ = ph.sb("b", [P, T])
        h_ = ph.sb("h", [P, T])
        hs_ = ph.sb("hs", [P, T])
        gel = ph.sb("gel", [P, TL])
        ob = ph.sb("ob", [P, TL], BF16)
        pss = [ph.ps("ps") for _ in range(4)]
        ip = 0
        for c in range(8):
            hh = c // 2
            x32 = xc[c % 2]
            S.dma(x32[:], self.d["lruXC"][c * 128:(c + 1) * 128, :])
            S.dma(gel[:], lruP[(8 + c) * 128:(9 + c) * 128, 0:TL])
            for d in range(2):
                for (t0, n, w) in ALL_TILES:
                    pr = pss[ip % 4]
                    pi = pss[(ip + 1) % 4]
                    ip += 2
                    for ic in range(2):
                        S.mm(pr[:, :n], wa[:, d * 4 + hh, ic, (c % 2) * 128:(c % 2) * 128 + 128], xcb[:, 2 * hh + ic, t0:t0 + n],
                             start=(ic == 0), stop=(ic == 1))
                    for ic in range(2):
                        S.mm(pi[:, :n], wx[:, d * 4 + hh, ic, (c % 2) * 128:(c % 2) * 128 + 128], xcb[:, 2 * hh + ic, t0:t0 + n],
                             start=(ic == 0), stop=(ic == 1))
                    S.act(r_[:, t0:t0 + n], pr[:, :n], AF.Sigmoid, bias=self.vcol("lru_ba", d * 8 + c))
                    S.act(i_[:, t0:t0 + n], pi[:, :n], AF.Sigmoid, bias=self.vcol("lru_bx", d * 8 + c))
                S.act(a_[:], r_[:], AF.Exp, scale=nsp[:, d * 8 + c:d * 8 + c + 1])
                S.tt("pool", b_[:], a_[:], a_[:], ALU.mult)
                S.act(b_[:], b_[:], AF.Sqrt, scale=-1.0, bias=self.c_one)
                S.tt("pool", b_[:], b_[:], i_[:], ALU.mult)
                S.tt("dve", b_[:], b_[:], x32[:], ALU.mult)
                if d == 0:
                    S.scan(h_[:, TL:T], a_[:, TL:T], b_[:, TL:T], 0.0, ALU.mult, ALU.add)
                    S.scan(hs_[:, 0:TL], a_[:, 0:TL], b_[:, 0:TL], h_[:, T - 1:T], ALU.mult, ALU.add)
                else:
                    S.scan(h_[:, T - 1:TL - 1:-1], a_[:, T - 1:TL - 1:-1], b_[:, T - 1:TL - 1:-1], 0.0, ALU.mult, ALU.add)
                    S.scan(h_[:, TL - 1::-1], a_[:, TL - 1::-1], b_[:, TL - 1::-1], h_[:, TL:TL + 1], ALU.mult, ALU.add)
                    S.tt("pool", hs_[:, 0:TL], hs_[:, 0:TL], h_[:, 0:TL], ALU.add)
            S.tt("dve", ob[:], hs_[:, 0:TL], gel[:], ALU.mult)
            S.dma(self.d["mixT"][c * 128:(c + 1) * 128, 0:TL], ob[:])
        ph.close()

    def declare(self):
        cfg = self.cfg
        self.inp("vec", [P, NV])
        self.inp("consts", [P, NCONST, P])
        self.inp("cc", [P, 16])
        self.inp("ada_w", [2, D, 6 * D])
        self.inp("ada_b_raw", [2, 6 * D])
        self.inp("xT0", [D, T])
        self.inp("posT", [D, TL])
        self.inp("ev_w_in", [D, 3600])
        self.scr("pT", [3600, T])
        self.inp("gparams", [16, 2])
        self.scr("qkvT", [1536, T])
        self.scr("gbT", [32, T])
        self.scr("dnO", [P, 18 * 4 * P])
        self.scr("mixT", [D, T], BF16)
        self.scr("hyX0", [512, T])
        self.inp("ev_w_out", [D, D])
        self.inp("od_w_out", [D, D])
        self.inp("router_w", [2, D, NE])
        self.inp("router_b", [2, NE])
        self.scr("x1T", [D, T])
        if self.cfg.get("xT1_input"):
            self.inp("xT1", [D, T])
        else:
            self.scr("xT1", [D, T])
        self.inp("od_w_in", [D, 2048])
        self.inp("lru_wa", [2, 4, 256, 256])
        self.inp("lru_wx", [2, 4, 256, 256])
        self.scr("lruP", [2048, T])
        self.scr("lruXC", [D, T])
        self.scr("hT", [D, T], BF16)
        self.scr("gateT", [NE, T])
        if self.cfg.get("moe", True):
            wl, we = self.cfg.get("moe_wshape", (2, NE))
            self.inp("moe_w1r", [wl, we, 2, P, 8, 1024])
            self.inp("moe_w2r", [wl, we, P, 8, 1024])
            self.inp("moe_b2", [2, NE, D])
        self.scr("out", [D, TL], out=True)
        self.scr("hyVX", [512, T])
        self.scr("hyVXtm", [T, 512], BF16)
        self.inp("hy_w_in", [33, 64])
        self.inp("hy_w_mid", [2, 64, 64])
        self.inp("hy_w_out", [64, 1024])
        self.inp("hy_negt", [P, 18])
        self.inp("hy_fsc", [P, 18])
        self.inp("hy_delt", [P, 512])
        self.inp("hy_altc", [P, 1], BF16)
        self.inp("hy_altr", [1, TL], BF16)
        self.inp("hy_zT_lat", [33, TL])
        self.inp("hy_zT_ctx", [33, TC])
        self.inp("dftC_lat", [TL, TL], BF16)
        self.inp("dftS_lat", [TL, TL], BF16)
        self.inp("dftC_ctx", [TC, TC], BF16)
        self.inp("dftS_ctx", [TC, TC], BF16)


def build_program(cfg):
    nc = bass.Bass("TRN2", target_bir_lowering=False)
    stack = contextlib.ExitStack()
    K = KB(nc, stack, cfg)
    K.declare()
    K.setup()
    for phn in cfg["phases"]:
        if isinstance(phn, tuple):
            getattr(K, "phase_" + phn[0])(*phn[1:])
        else:
            getattr(K, "phase_" + phn)()
    K.S.barrier()
    stack.close()
    return nc, K


_CONST_CACHE = {}


def host_consts():
    if not _CONST_CACHE:
        _CONST_CACHE["consts"] = make_consts()
        _CONST_CACHE["posT"] = make_pos()
        zl, tl = make_hy_z(TL)
        zc, tcx = make_hy_z(TC)
        _CONST_CACHE["hy_zT_lat"] = zl
        _CONST_CACHE["hy_zT_ctx"] = zc
        negt = np.zeros((P, 18), np.float32)
        negt[:, 0:16] = -tl.reshape(16, P).T
        negt[:, 16:18] = -tcx.reshape(2, P).T
        _CONST_CACHE["hy_negt"] = negt
        fsc = np.zeros((P, 18), np.float32)
        fsc[:, 0:16] = 2.0 / (2 * TL)
        fsc[0, 0] = 1.0 / (2 * TL)
        fsc[:, 16:18] = 2.0 / (2 * TC)
        fsc[0, 16] = 1.0 / (2 * TC)
        _CONST_CACHE["hy_fsc"] = fsc
        deltas = np.abs(np.linspace(HY_MIN_DECAY, HY_MAX_DECAY, 512, dtype=np.float32))
        _CONST_CACHE["hy_delt"] = np.ascontiguousarray(np.broadcast_to(deltas[None, :], (P, 512))).astype(np.float32)
        alt = ((-1.0) ** np.arange(TL)).astype(np.float32)
        _CONST_CACHE["hy_altc"] = alt[:P].reshape(P, 1).astype(ml_dtypes.bfloat16)
        _CONST_CACHE["hy_altr"] = alt.reshape(1, TL).astype(ml_dtypes.bfloat16)
        c, s_ = make_dft(TL)
        _CONST_CACHE["dftC_lat"] = c
        _CONST_CACHE["dftS_lat"] = s_
        c, s_ = make_dft(TC)
        _CONST_CACHE["dftC_ctx"] = c
        _CONST_CACHE["dftS_ctx"] = s_
    return _CONST_CACHE


def prep_shared(inputs):
    g = {k: np.asarray(v) for k, v in inputs.items()}
    vec = np.zeros((P, NV), np.float32)

    def put(name, i, v):
        c = _cols(v)
        o = VEC_OFF[name] + i
        vec[:, o:o + c.shape[1]] = c
    for l in range(2):
        put("ada_b", l * 48, g["ada_b"][l])
        for w in range(2):
            put("ln_g", (l * 2 + w) * 8, g["ln_g"][l, w])
            put("ln_b", (l * 2 + w) * 8, g["ln_b"][l, w])
    for tp in range(4):
        put("dn_conv_w", tp * 12, g["dn_conv_w"][0, tp])
        put("lru_conv_w", tp * 8, g["lru_conv_w"][0, tp])
    for tp in range(3):
        put("hy_conv_w", tp * 12, g["hy_conv_w"][0, tp])
    put("hy_conv_b", 0, g["hy_conv_b"][0])
    put("hy_skip", 0, g["hy_skip"][0])
    put("od_b_in", 0, g["od_b_in"][0])
    put("lru_conv_b", 0, g["lru_conv_b"][0])
    for d in range(2):
        put("lru_ba", d * 8, g["lru_ba"][0, d])
        put("lru_bx", d * 8, g["lru_bx"][0, d])
        put("lru_a", d * 8, g["lru_a_param"][0, d])
    put("od_b_out", 0, g["od_b_out"][0])
    put("dn_norm_g", 0, g["dn_norm_g"][0])
    o = VEC_OFF["hy_mlp"]
    vec[:64, o + 0] = g["hy_b_in"][0]
    vec[:64, o + 1] = g["hy_b_mid"][0, 0]
    vec[:64, o + 2] = g["hy_b_mid"][0, 1]
    vec[:64, o + 3] = g["hy_freq"][0]
    b1 = g["moe_b1"]
    b1d = np.concatenate([b1[..., 0::2], b1[..., 1::2]], axis=-1)
    o = VEC_OFF["moe_b1"]
    vec[:, o:o + 1024] = b1d.reshape(2 * 32 * 16, P).T
    gparams = np.zeros((16, 2), np.float32)
    for d in range(2):
        gparams[d * 8:d * 8 + 4, 0] = g["dn_dt_bias"][0, d]
        gparams[d * 8:d * 8 + 4, 1] = g["dn_a_log"][0, d]
    sh = {"vec": vec, "ada_w": g["ada_w"], "ada_b_raw": g["ada_b"], "ev_w_in": g["ev_w_in"][0], "gparams": gparams,
          "hy_w_in": g["hy_w_in"][0], "hy_w_mid": g["hy_w_mid"][0], "hy_w_out": g["hy_w_out"][0],
          "ev_w_out": g["ev_w_out"][0], "od_w_out": g["od_w_out"][0], "router_w": g["router_w"], "router_b": g["router_b"],
          "od_w_in": g["od_w_in"][0], "lru_wa": g["lru_wa"][0], "lru_wx": g["lru_wx"][0]}
    if "moe_w1" in g:
        w1 = g["moe_w1"].reshape(2, NE, 8, P, 2, 4, P, 2)
        sh["moe_w1r"] = np.ascontiguousarray(w1.transpose(0, 1, 4, 3, 2, 5, 7, 6)).reshape(2, NE, 2, P, 8, 1024)
        w2 = g["moe_w2"].reshape(2, NE, 8, P, D)
        sh["moe_w2r"] = np.ascontiguousarray(w2.transpose(0, 1, 3, 2, 4))
        sh["moe_b2"] = g["moe_b2"]
    sh.update(host_consts())
    return sh


def prep_core(inputs, b):
    x = np.asarray(inputs["x"][b])
    ctx = np.asarray(inputs["ctx"][b])
    xT0 = np.ascontiguousarray(np.concatenate([x, ctx], axis=0).T)
    cc = np.stack([np.asarray(inputs["c"][b]), np.asarray(inputs["c_ctx"])], axis=-1)
    cc = np.ascontiguousarray(cc.reshape(8, P, 2).transpose(1, 0, 2).reshape(P, 16))
    return {"xT0": xT0, "cc": cc}


FULL_PHASES = ["mod", "l0_proj", "dn_prep", "dn_core", "dn_post", "hy_prep", "hy_main", ("post", 0), ("moe", 0),
               "l1_proj", "lru", ("post", 1), ("moe", 1)]


def kernel(**inputs):
    cfg = {"phases": FULL_PHASES, "dump": []}
    nc, K = build_program(cfg)
    sh = prep_shared(inputs)
    in_names = [n for n in K.d if n in sh or n in ("xT0", "cc")]
    maps = []
    for b in range(8):
        m = dict(sh)
        m.update(prep_core(inputs, b))
        maps.append({k: m[k] for k in in_names})
    res = run_bass_kernel_spmd(nc, maps, core_ids=list(range(8)))
    out = np.stack([np.ascontiguousarray(np.asarray(r["out"]).T) for r in res.results], axis=0)
    return out.astype(np.float32)
```

```python
import math
import contextlib
import numpy as np
import ml_dtypes
import concourse.bass as bass
import concourse.mybir as mybir
from concourse.bass_utils import run_bass_kernel_spmd

F32 = mybir.dt.float32
BF16 = mybir.dt.bfloat16
AF = mybir.ActivationFunctionType
ALU = mybir.AluOpType
AX = mybir.AxisListType

P = 128
D = 1024
KC = 8
TL = 2048
TC = 256
T = TL + TC
NE = 32
ALPHA = 4.0 ** 0.25
LN_EPS = 1e-5
RMS_EPS = 1e-6
N_DMA_SEMS = 40
EPOCH = 30000


def _region(ap):
    t = ap.tensor
    dims = list(ap.ap)
    tn = type(t).__name__
    if "DRam" in tn:
        lo = ap.offset
        hi = ap.offset
        for st, cnt in dims:
            if st >= 0:
                hi += st * (cnt - 1)
            else:
                lo += st * (cnt - 1)
        return (t.name, 0, 1, lo, hi + 1)
    if "PSum" in tn or "Psum" in tn or "PSUM" in tn:
        return (t.name, 0, 128, 0, 1 << 30)
    F = 1
    for s in list(t.shape)[1:]:
        F *= s
    p0 = ap.offset // F
    f0 = ap.offset % F
    pst, pcnt = dims[0]
    npart = 1 if pst == 0 else pcnt
    lo = f0
    hi = f0
    for st, cnt in dims[1:]:
        if st >= 0:
            hi += st * (cnt - 1)
        else:
            lo += st * (cnt - 1)
    return (t.name, p0, p0 + npart, lo, hi + 1)


def _overlap(a, b):
    return a[1] < b[2] and b[1] < a[2] and a[3] < b[4] and b[3] < a[4]


def _covers(a, b):
    return a[1] <= b[1] and a[2] >= b[2] and a[3] <= b[3] and a[4] >= b[4]


class _Op:
    __slots__ = ("eng", "fn", "deps", "signal", "dma", "idx", "sem", "target")

    def __init__(self, eng, fn, dma):
        self.eng = eng
        self.fn = fn
        self.deps = ()
        self.signal = False
        self.dma = dma
        self.sem = None
        self.target = None


class Sched:
    ENGS = ("pe", "dve", "act", "pool", "sp")

    def __init__(self, nc, stack):
        self.nc = nc
        self.stack = stack
        self.ops = []
        self.h = {"pe": nc.tensor, "dve": nc.vector, "act": nc.scalar, "pool": nc.gpsimd, "sp": nc.sync}
        self.rec = {}
        self.emitted = 0
        self.sems = {e: [] for e in self.ENGS}
        self.dma_sems = [stack.enter_context(nc.semaphore("sdma%d" % k)) for k in range(N_DMA_SEMS)]
        self.dma_cnt = [0] * N_DMA_SEMS
        self.dma_last = [None] * N_DMA_SEMS
        self.dma_rr = 0
        self.cnt = {e: 0 for e in self.ENGS}
        self.known = {e: {p: 0 for p in self.ENGS} for e in self.ENGS}
        self.known_dma = {e: set() for e in self.ENGS}
        self.n_wait = 0

    def _sem(self, eng, ep):
        lst = self.sems[eng]
        while len(lst) <= ep:
            lst.append(self.stack.enter_context(self.nc.semaphore("s%s%d" % (eng, len(lst)))))
        return lst[ep]

    def op(self, eng, fn, reads, writes, dma=False):
        o = _Op(eng, fn, dma)
        o.idx = len(self.ops)
        self.ops.append(o)
        deps = set()
        for ap in reads:
            r = _region(ap)
            ent = self.rec.setdefault(r[0], ([], []))
            for rr, oi in ent[0]:
                if _overlap(r, rr):
                    deps.add(oi)
            ent[1].append((r, o.idx))
        for ap in writes:
            r = _region(ap)
            ent = self.rec.setdefault(r[0], ([], []))
            for k in (0, 1):
                keep = []
                for rr, oi in ent[k]:
                    if oi != o.idx and _overlap(r, rr):
                        deps.add(oi)
                        if _covers(r, rr):
                            continue
                    keep.append((rr, oi))
                ent[k][:] = keep
            ent[0].append((r, o.idx))
        deps.discard(o.idx)
        o.deps = tuple(sorted(deps))
        return o

    def _wait_compute(self, eng, pe, tgt):
        if self.known[eng][pe] >= tgt:
            return
        self.known[eng][pe] = tgt
        ep = (tgt - 1) // EPOCH
        self.h[eng].wait_ge(self._sem(pe, ep), tgt - ep * EPOCH)
        self.n_wait += 1

    def _wait_dma(self, eng, d):
        if d in self.known_dma[eng]:
            return
        self.known_dma[eng].add(d)
        p = self.ops[d]
        self.h[eng].wait_ge(p.sem, p.target)
        self.n_wait += 1

    def flush(self, final=False):
        ops = self.ops
        new = ops[self.emitted:]
        for o in new:
            for d in o.deps:
                p = ops[d]
                if p.dma:
                    continue
                if p.eng == o.eng and o.eng == "pe":
                    continue
                p.signal = True
        if final:
            last = {}
            for o in new:
                if not o.dma:
                    last[o.eng] = o
            for o in last.values():
                o.signal = True
        for o in new:
            h = self.h[o.eng]
            need = {}
            for d in o.deps:
                p = ops[d]
                if p.dma:
                    self._wait_dma(o.eng, d)
                else:
                    if p.eng == o.eng and o.eng == "pe":
                        continue
                    if p.target is None:
                        raise RuntimeError("dep on unsignalled op")
                    if p.target > need.get(p.eng, 0):
                        need[p.eng] = p.target
            for pe, tgt in need.items():
                self._wait_compute(o.eng, pe, tgt)
            if o.dma:
                k = self.dma_rr % N_DMA_SEMS
                self.dma_rr += 1
                if self.dma_last[k] is not None:
                    self._wait_dma(o.eng, self.dma_last[k])
                self.dma_cnt[k] += 16
                o.sem = self.dma_sems[k]
                o.target = self.dma_cnt[k]
                self.dma_last[k] = o.idx
                o.fn(h).then_inc(o.sem, 16)
            else:
                ins = o.fn(h)
                if o.signal:
                    self.cnt[o.eng] += 1
                    o.target = self.cnt[o.eng]
                    ep = (o.target - 1) // EPOCH
                    ins.then_inc(self._sem(o.eng, ep), 1)
            o.fn = None
        self.emitted = len(ops)

    def barrier(self):
        self.flush(final=True)
        for e in self.ENGS:
            for pe in self.ENGS:
                if pe != e and self.cnt[pe] > 0:
                    self._wait_compute(e, pe, self.cnt[pe])
            for k in range(N_DMA_SEMS):
                if self.dma_last[k] is not None:
                    self._wait_dma(e, self.dma_last[k])
        self.rec = {}

    def dma(self, out, in_, eng="sp", **kw):
        return self.op(eng, lambda e: e.dma_start(out=out, in_=in_, **kw), [in_], [out], dma=True)

    def mm(self, out, lhsT, rhs, start=True, stop=True, **kw):
        return self.op("pe", lambda e: e.matmul(out, lhsT, rhs, start=start, stop=stop, **kw), [lhsT, rhs], [out])

    def tr(self, out, in_, ident):
        return self.op("pe", lambda e: e.transpose(out, in_, ident), [in_, ident], [out])

    def act(self, out, in_, func, bias=None, scale=None, accum_out=None):
        kw = {}
        rd = [in_]
        wr = [out]
        if bias is not None:
            kw["bias"] = bias
            if not isinstance(bias, (int, float)):
                rd.append(bias)
        if scale is not None:
            kw["scale"] = scale
            if not isinstance(scale, (int, float)):
                rd.append(scale)
        if accum_out is not None:
            kw["accum_out"] = accum_out
            wr.append(accum_out)
        return self.op("act", lambda e: e.activation(out, in_, func, **kw), rd, wr)

    def ts(self, eng, out, in0, s1, s2, op0, op1=None, accum_out=None):
        rd = [in0]
        wr = [out]
        for s in (s1, s2):
            if s is not None and not isinstance(s, (int, float)):
                rd.append(s)
        kw = {}
        if accum_out is not None:
            kw["accum_out"] = accum_out
            wr.append(accum_out)
        if op1 is None:
            return self.op(eng, lambda e: e.tensor_scalar(out, in0, s1, None, op0, **kw), rd, wr)
        return self.op(eng, lambda e: e.tensor_scalar(out, in0, s1, s2, op0, op1, **kw), rd, wr)

    def tt(self, eng, out, in0, in1, op):
        return self.op(eng, lambda e: e.tensor_tensor(out, in0, in1, op), [in0, in1], [out])

    def stt(self, out, in0, scalar, in1, op0, op1, accum_out=None):
        rd = [in0, in1]
        wr = [out]
        if not isinstance(scalar, (int, float)):
            rd.append(scalar)
        kw = {}
        if accum_out is not None:
            kw["accum_out"] = accum_out
            wr.append(accum_out)
        return self.op("dve", lambda e: e.scalar_tensor_tensor(out, in0, scalar, in1, op0, op1, **kw), rd, wr)

    def copy(self, eng, out, in_):
        if eng == "act":
            return self.op(eng, lambda e: e.copy(out, in_), [in_], [out])
        return self.op(eng, lambda e: e.tensor_copy(out, in_), [in_], [out])

    def memset(self, eng, ap, val):
        return self.op(eng, lambda e: e.memset(ap, val), [], [ap])

    def scan(self, out, d0, d1, init, op0, op1):
        rd = [d0, d1]
        if not isinstance(init, (int, float)):
            rd.append(init)
        return self.op("dve", lambda e: e.tensor_tensor_scan(out, d0, d1, init, op0, op1), rd, [out])

    def recip(self, out, in_):
        return self.op("dve", lambda e: e.reciprocal(out, in_), [in_], [out])

    def reduce(self, out, in_, axis, op):
        return self.op("dve", lambda e: e.tensor_reduce(out, in_, axis, op), [in_], [out])

    def max8(self, out, in_):
        return self.op("dve", lambda e: e.max(out, in_), [in_], [out])


class Phase:
    def __init__(self, K, name):
        self.K = K
        self.name = name
        self.st = contextlib.ExitStack()
        self.n = 0

    def sb(self, name, shape, dtype=F32):
        self.n += 1
        return self.st.enter_context(self.K.nc.sbuf_tensor("%s_%s_%d" % (self.name, name, self.n), list(shape), dtype))

    def ps(self, name, dtype=F32, cols=512):
        self.n += 1
        return self.st.enter_context(self.K.nc.psum_tensor("%s_%s_%d" % (self.name, name, self.n), [P, cols], dtype))

    def close(self):
        self.K.S.barrier()
        self.st.close()
        if not hasattr(self.K, "marks"):
            self.K.marks = []
        self.K.marks.append((self.name, sum(1 for o in self.K.S.ops if o.dma and o.eng == "sp")))


VEC_ITEMS = [("ada_b", 96), ("ln_g", 32), ("ln_b", 32), ("dn_conv_w", 48), ("hy_conv_w", 36), ("hy_conv_b", 12),
             ("hy_skip", 4), ("od_b_in", 16), ("lru_conv_w", 32), ("lru_conv_b", 8), ("lru_ba", 16), ("lru_bx", 16),
             ("lru_a", 16), ("od_b_out", 8), ("dn_norm_g", 1), ("hy_mlp", 4), ("moe_b1", 1024)]
VEC_OFF = {}
_o = 0
for _n, _c in VEC_ITEMS:
    VEC_OFF[_n] = _o
    _o += _c
NV = _o

C_ID, C_ONES = 0, 1


def C_L1(d):
    return 2 + d * 11


def C_U1(d):
    return 3 + d * 11


def C_STRICT(d):
    return 4 + d * 11


def C_INCLT(d):
    return 5 + d * 11


def C_E(d, lvl):
    return 6 + d * 11 + lvl


NCONST = 24


def _cols(v):
    v = np.asarray(v, np.float32).reshape(-1, P)
    return np.ascontiguousarray(v.T)


def make_consts():
    c = np.zeros((P, NCONST, P), np.float32)
    i = np.arange(P)[:, None]
    j = np.arange(P)[None, :]
    c[:, C_ID] = (i == j)
    c[:, C_ONES] = 1.0
    for d in range(2):
        if d == 0:
            c[:, C_L1(d)] = (i <= j)
            c[:, C_U1(d)] = (i > j)
            c[:, C_STRICT(d)] = (i > j)
            c[:, C_INCLT(d)] = (j >= i)
        else:
            c[:, C_L1(d)] = (i >= j)
            c[:, C_U1(d)] = (i < j)
            c[:, C_STRICT(d)] = (i < j)
            c[:, C_INCLT(d)] = (j <= i)
        for lvl in range(7):
            b = 1 << lvl
            same = (i // (2 * b)) == (j // (2 * b))
            ih = (i % (2 * b)) >= b
            jh = (j % (2 * b)) >= b
            if d == 0:
                c[:, C_E(d, lvl)] = same & ih & (~jh)
            else:
                c[:, C_E(d, lvl)] = same & (~ih) & jh
    return c


def make_pos():
    quarter = D // 4
    omega = (1.0 / (10000.0 ** (np.arange(quarter, dtype=np.float32) / np.float32(quarter)))).astype(np.float32)

    def emb1d(n):
        ang = (np.arange(n, dtype=np.float32)[:, None] * omega).astype(np.float32)
        return np.concatenate([np.sin(ang), np.cos(ang)], axis=-1).astype(np.float32)
    rows, colsn = TL // 64, 64
    er = np.broadcast_to(emb1d(rows)[:, None], (rows, colsn, D // 2))
    ec = np.broadcast_to(emb1d(colsn)[None], (rows, colsn, D // 2))
    pos = np.concatenate([er, ec], axis=-1).reshape(rows * colsn, D)
    return np.ascontiguousarray(pos.T).astype(np.float32)


def make_hy_z(length):
    t = np.linspace(0.0, 1.0, length, dtype=np.float32)[:, None]
    bands = 16
    wpos = (2.0 * math.pi * np.arange(length, dtype=np.float32)[:, None] / length).astype(np.float32)
    fb = np.linspace(1e-4, bands - 1, bands, dtype=np.float32)[None]
    z = np.concatenate([t, np.cos(fb * wpos), -np.sin(fb * wpos)], axis=-1).astype(np.float32)
    return np.ascontiguousarray(z.T), t[:, 0]


def make_dft(L):
    n = 2 * L
    k = (np.arange(L, dtype=np.int64)[:, None] * np.arange(L, dtype=np.int64)[None, :]) % n
    ang = 2.0 * math.pi * k.astype(np.float64) / n
    return np.cos(ang).astype(ml_dtypes.bfloat16), np.sin(ang).astype(ml_dtypes.bfloat16)


HY_MIN_DECAY = math.log(1e-2) / 1.5
HY_MAX_DECAY = math.log(1e-2) / 0.3


LAT_TILES = [(0, 512), (512, 512), (1024, 512), (1536, 512)]
ALL_TILES = [(0, 512, 0), (512, 512, 0), (1024, 512, 0), (1536, 512, 0), (2048, 256, 1)]


class KB:
    def __init__(self, nc, stack, cfg):
        self.nc = nc
        self.cfg = cfg
        self.S = Sched(nc, stack)
        self.stack = stack
        self.d = {}
        self.dump = set(cfg.get("dump", ()))
        self.outs = []

    def inp(self, name, shape, dtype=F32):
        self.d[name] = self.nc.dram_tensor(name, list(shape), dtype, kind="ExternalInput").ap()
        return self.d[name]

    def scr(self, name, shape, dtype=F32, out=False):
        if name in self.cfg.get("scr_inputs", ()):
            return self.inp(name, shape, dtype)
        kind = "ExternalOutput" if (out or name in self.dump) else "Internal"
        if kind == "ExternalOutput":
            self.outs.append(name)
        self.d[name] = self.nc.dram_tensor(name, list(shape), dtype, kind=kind).ap()
        return self.d[name]

    def vcol(self, name, i):
        o = VEC_OFF[name] + i
        return self.vec[:, o:o + 1]

    def cst(self, i):
        return self.consts[:, i, :]

    def setup(self):
        nc, S, st = self.nc, self.S, self.stack
        self.vec = st.enter_context(nc.sbuf_tensor("vec_sb", [P, NV], F32))
        self.consts = st.enter_context(nc.sbuf_tensor("consts_sb", [P, NCONST, P], F32))
        self.mod = st.enter_context(nc.sbuf_tensor("mod_sb", [P, 2, 48, 2], F32))
        self.onep = st.enter_context(nc.sbuf_tensor("onep", [P, 2, 2, 8, 2], F32))
        self.kc_ = st.enter_context(nc.sbuf_tensor("kconst", [P, 8], F32))
        S.dma(self.vec[:], self.d["vec"])
        S.dma(self.consts[:], self.d["consts"])
        vals = [1.0, LN_EPS, RMS_EPS, 0.0, -1.0, math.pi, 1e-30, 0.5]
        for i, v in enumerate(vals):
            S.memset("pool", self.kc_[:, i:i + 1], v)
        b1o = VEC_OFF["moe_b1"]
        b1v = self.vec[:, b1o:b1o + 1024].rearrange("p (a g j) -> p a g j", g=2, j=8)
        S.ts("dve", b1v[:, :, 1, :], b1v[:, :, 1, :], 1.0, None, ALU.add)
        self.c_one = self.kc_[:, 0:1]
        self.c_lneps = self.kc_[:, 1:2]
        self.c_rmseps = self.kc_[:, 2:3]
        self.c_zero = self.kc_[:, 3:4]

    def phase_mod(self):
        S = self.S
        ph = Phase(self, "mod")
        cc = ph.sb("cc", [P, 16])
        sc = ph.sb("sc", [P, 16])
        S.dma(cc[:], self.d["cc"])
        S.act(sc[:], cc[:], AF.Silu)
        ident = self.cst(C_ID)
        psA = [ph.ps("psA") for _ in range(2)]
        psB = [ph.ps("psB") for _ in range(2)]
        psT = [ph.ps("psT") for _ in range(2)]
        wb = [ph.sb("w", [P, 8, 768]) for _ in range(2)]
        rows = [ph.sb("row", [2, 768]) for _ in range(2)]
        bb = ph.sb("bb", [2, 2, 6 * D])
        for l in range(2):
            S.dma(bb[:, l, :], self.d["ada_b_raw"][l].partition_broadcast(2))
        it = 0
        for l in range(2):
            src = self.d["ada_w"][l].rearrange("(kc p) j -> p kc j", p=P)
            for jb in range(8):
                w = wb[it % 2]
                pA, pB, pT, row = psA[it % 2], psB[it % 2], psT[it % 2], rows[it % 2]
                it += 1
                S.dma(w[:], src[:, :, jb * 768:(jb + 1) * 768])
                for kc in range(8):
                    S.mm(pA[0:2, 0:512], sc[:, 2 * kc:2 * kc + 2], w[:, kc, 0:512], start=(kc == 0), stop=(kc == 7))
                for kc in range(8):
                    S.mm(pB[0:2, 0:256], sc[:, 2 * kc:2 * kc + 2], w[:, kc, 512:768], start=(kc == 0), stop=(kc == 7))
                S.tt("dve", row[:, 0:512], pA[0:2, 0:512], bb[:, l, jb * 768:jb * 768 + 512], ALU.add)
                S.tt("dve", row[:, 512:768], pB[0:2, 0:256], bb[:, l, jb * 768 + 512:(jb + 1) * 768], ALU.add)
                for jj in range(6):
                    S.tr(pT[:, 2 * jj:2 * jj + 2], row[0:2, jj * 128:(jj + 1) * 128], ident[:2, :2])
                S.copy("act", self.mod[:, l, jb * 6:(jb + 1) * 6, :], pT[:, 0:12].rearrange("p (a b) -> p a b", b=2))
            S.ts("pool", self.onep[:, l, 0, :, :], self.mod[:, l, 8:16, :], 1.0, None, ALU.add)
            S.ts("pool", self.onep[:, l, 1, :, :], self.mod[:, l, 32:40, :], 1.0, None, ALU.add)
        if "mod" in self.dump:
            o = self.scr("mod", [P, 2 * 48 * 2])
            S.dma(o, self.mod[:].rearrange("p a b c -> p (a b c)"))
        ph.close()

    def m_sh1(self, l, kc, w):
        return self.mod[:, l, 0 + kc, w:w + 1]

    def m_g1(self, l, kc, w):
        return self.mod[:, l, 16 + kc, w:w + 1]

    def m_sh2(self, l, kc, w):
        return self.mod[:, l, 24 + kc, w:w + 1]

    def m_g2(self, l, kc, w):
        return self.mod[:, l, 40 + kc, w:w + 1]

    def m_a1(self, l, kc, w):
        return self.onep[:, l, 0, kc, w:w + 1]

    def m_a2(self, l, kc, w):
        return self.onep[:, l, 1, kc, w:w + 1]

    def make_u(self, ph, l, xsrc, t0, n, w, xt, pt, u):
        S = self.S
        S.dma(xt[:, :, :n], xsrc.rearrange("(kc p) t -> p kc t", p=P)[:, :, t0:t0 + n])
        if w == 0:
            S.dma(pt[:, :, :n], self.d["posT"].rearrange("(kc p) t -> p kc t", p=P)[:, :, t0:t0 + n])
        for kc in range(8):
            if w == 0:
                S.stt(pt[:, kc, :n], xt[:, kc, :n], self.m_a1(l, kc, 0), pt[:, kc, :n], ALU.mult, ALU.add)
                S.act(u[:, kc, :n], pt[:, kc, :n], AF.Identity, bias=self.m_sh1(l, kc, 0))
            else:
                S.ts("dve", u[:, kc, :n], xt[:, kc, :n], self.m_a1(l, kc, 1), self.m_sh1(l, kc, 1), ALU.mult, ALU.add)

    def phase_l0_proj(self):
        S = self.S
        ph = Phase(self, "l0a")
        wsb = ph.sb("w", [P, 8, 3600], BF16)
        src = self.d["ev_w_in"].rearrange("(kc p) n -> p kc n", p=P)
        for q in range(4):
            S.dma(wsb[:, 2 * q:2 * q + 2, :], src[:, 2 * q:2 * q + 2, :], eng="pool")
        xts = [ph.sb("xt", [P, 8, 512]) for _ in range(2)]
        pts = [ph.sb("pt", [P, 8, 512]) for _ in range(2)]
        us = [ph.sb("u", [P, 8, 512], BF16) for _ in range(2)]
        stg = [ph.sb("stg", [P, 4, 512]) for _ in range(3)]
        pss = [ph.ps("ps") for _ in range(4)]
        pT = self.d["pT"]
        ip = 0
        ist = 0
        for ti, (t0, n, w) in enumerate(ALL_TILES):
            xt, pt, u = xts[ti % 2], pts[ti % 2], us[ti % 2]
            self.make_u(ph, 0, self.d["xT0"], t0, n, w, xt, pt, u)
            for m0 in range(0, 29, 4):
                sg = stg[ist % 3]
                ist += 1
                ms = list(range(m0, min(m0 + 4, 29)))
                for m in ms:
                    if m < 16:
                        c0, wd = m * 128, 128
                    elif m < 28:
                        c0, wd = 2064 + (m - 16) * 128, 128
                    else:
                        c0, wd = 2048, 16
                    p = pss[ip % 4]
                    ip += 1
                    for kc in range(8):
                        S.mm(p[:wd, :n], wsb[:, kc, c0:c0 + wd], u[:, kc, :n], start=(kc == 0), stop=(kc == 7))
                    if ip % 2 == 0:
                        S.copy("act", sg[:wd, m - m0, :n], p[:wd, :n])
                    else:
                        S.copy("dve", sg[:wd, m - m0, :n], p[:wd, :n])
                if ms[-1] < 28:
                    dst = pT[m0 * 128:(m0 + 4) * 128, t0:t0 + n].rearrange("(m p) t -> p m t", p=P)
                    S.dma(dst, sg[:, :, :n])
                else:
                    S.dma(pT[3584:3600, t0:t0 + n], sg[:16, 0, :n])
        ph.close()

    def conv_fm(self, out, x, wcol, taps, pad_left, segs, bias=None):
        S = self.S
        for (t0, n) in segs:
            o = out[:, t0:t0 + n]
            xi = x[:, t0:t0 + n]
            if bias is None:
                S.act(o, xi, AF.Copy, scale=wcol(pad_left))
            else:
                S.act(o, xi, AF.Identity, scale=wcol(pad_left), bias=bias)
            for i in range(taps):
                if i == pad_left:
                    continue
                s = i - pad_left
                if s < 0:
                    S.stt(out[:, t0 - s:t0 + n], x[:, t0:t0 + n + s], wcol(i), out[:, t0 - s:t0 + n], ALU.mult, ALU.add)
                else:
                    S.stt(out[:, t0:t0 + n - s], x[:, t0 + s:t0 + n], wcol(i), out[:, t0:t0 + n - s], ALU.mult, ALU.add)


    def phase_dn_prep(self):
        S = self.S
        ph = Phase(self, "dnp")
        pT = self.d["pT"]
        qkvT = self.d["qkvT"]
        segs = [(0, TL), (TL, TC)]
        ones = self.cst(C_ONES)
        xin = [ph.sb("xin", [P, T]) for _ in range(2)]
        cv = [ph.sb("cv", [P, T]) for _ in range(2)]
        sv = [ph.sb("sv", [P, T]) for _ in range(2)]
        sq = [ph.sb("sq", [P, T]) for _ in range(2)]
        rs = [ph.sb("rs", [P, 512]) for _ in range(2)]
        pss = [ph.ps("ps") for _ in range(2)]
        ip = 0
        for m in range(12):
            a, c, s_, q_ = xin[m % 2], cv[m % 2], sv[m % 2], sq[m % 2]
            S.dma(a[:], pT[m * 128:(m + 1) * 128, :])
            self.conv_fm(c, a, lambda i, m=m: self.vcol("dn_conv_w", i * 12 + m), 4, 2, segs)
            S.act(s_[:], c[:], AF.Silu)
            if m < 8:
                S.act(q_[:], s_[:], AF.Square)
                for (t0, n, w) in ALL_TILES:
                    p = pss[ip % 2]
                    r = rs[ip % 2]
                    ip += 1
                    S.mm(p[:, :n], ones, q_[:, t0:t0 + n])
                    S.act(r[:, :n], p[:, :n], AF.Sqrt, bias=self.c_rmseps)
                    S.recip(r[:, :n], r[:, :n])
                    scl = (128.0 ** -0.5) if m < 4 else 1.0
                    S.stt(s_[:, t0:t0 + n], s_[:, t0:t0 + n], scl, r[:, :n], ALU.mult, ALU.mult)
            S.dma(qkvT[m * 128:(m + 1) * 128, :], s_[:])
        gt = ph.sb("gt", [16, T])
        gp = ph.sb("gp", [16, 2])
        na = ph.sb("na", [16, 1])
        e1 = ph.sb("e1", [16, T])
        bt = ph.sb("bt", [16, T])
        S.dma(gt[:], pT[3584:3600, :])
        S.dma(gp[:], self.d["gparams"])
        S.act(na[:], gp[:, 1:2], AF.Exp)
        S.ts("dve", na[:], na[:], -1.0, None, ALU.mult)
        S.act(e1[:], gt[:], AF.Exp, bias=gp[:, 0:1])
        S.act(e1[:], e1[:], AF.Ln, bias=self.kc_[:16, 0:1])
        S.ts("dve", e1[:], e1[:], na[:, 0:1], None, ALU.mult)
        S.act(bt[:], gt[:], AF.Sigmoid)
        S.dma(self.d["gbT"][0:16, :], e1[:])
        S.dma(self.d["gbT"][16:32, :], bt[:])
        ph.close()

    def phase_dn_core(self):
        S = self.S
        ph = Phase(self, "dnc")
        F32R = mybir.dt.float32r

        def r(ap):
            return ap
        qkvT = self.d["qkvT"].rearrange("(m p) t -> p m t", p=P)
        gbT = self.d["gbT"]
        ident = self.cst(C_ID)
        ones = self.cst(C_ONES)
        NCH = T // P
        order = [[16, 17] + list(range(16)), [17, 16] + list(range(15, -1, -1))]
        step_of = [{c: s for s, c in enumerate(order[d])} for d in range(2)]
        O = ph.sb("O", [P, NCH, 4, P])
        zero_t = ph.sb("zero", [P, P])
        S.memset("pool", zero_t[:], 0.0)
        Sst = [[[ph.sb("S", [P, P]) for _ in range(2)] for h in range(4)] for d in range(2)]
        Sbf = [[[ph.sb("Sb", [P, P], BF16) for _ in range(2)] for h in range(4)] for d in range(2)]
        for d in range(2):
            for h in range(4):
                S.copy("dve", Sst[d][h][0][:], zero_t[:])
                S.copy("dve", Sbf[d][h][0][:], zero_t[:])
        qkv = [ph.sb("qkv", [P, 12, P]) for d in range(2)]
        qkr = [[ph.sb("qkr", [P, 8, P], BF16) for _ in range(2)] for d in range(2)]
        gb32 = [[ph.sb("gb32", [32, P]) for _ in range(2)] for d in range(2)]
        gtm = [[ph.sb("gtm", [P, 32]) for _ in range(2)] for d in range(2)]
        gs = [[ph.sb("gs", [P, 24]) for _ in range(2)] for d in range(2)]
        ktm = [[ph.sb("ktm", [P, 4, P]) for _ in range(2)] for d in range(2)]
        vtm = [[ph.sb("vtm", [P, 4, P]) for _ in range(2)] for d in range(2)]
        names_out = ["R", "WnT", "attnT", "Vb", "Kdec"]
        outb = [[[{n: ph.sb(n, [P, P], BF16) for n in names_out} for _ in range(2)] for h in range(4)] for d in range(2)]
        names_tmp = ["gU", "A", "Dt", "E0f", "av", "ot"]
        names_tmpb = ["E0", "E1", "Kbg", "X0", "X1", "Xt0", "Xt1", "H", "vn"]
        tmpb = [[{n: ph.sb(n, [P, P]) for n in names_tmp} for h in range(4)] for d in range(2)]
        for d in range(2):
            for h in range(4):
                for n in names_tmpb:
                    tmpb[d][h][n] = ph.sb(n, [P, P], BF16)
        for d in range(2):
            for h in range(4):
                tmpb[d][h]["Dm"] = tmpb[d][h]["gU"]
        ps_pa = [ph.ps("pa") for _ in range(1)]
        ps_rec = [ph.ps("rec") for _ in range(4)]
        ps_seq = [ph.ps("seq") for _ in range(2)]
        ps_misc = ph.ps("misc")
        cnt = {"pa": 0, "rec": 0, "seq": 0}

        def nxt(kind, lst):
            p = lst[cnt[kind] % len(lst)]
            cnt[kind] += 1
            return p

        def interleave(lists):
            n = max(len(x) for x in lists)
            for i in range(n):
                for x in lists:
                    if i < len(x):
                        x[i]()

        def prep_shared_d(s, d):
            c = order[d][s]
            par = s % 2
            qk = qkv[d]
            qr = qkr[d][par]
            S.dma(qk[:], qkvT[:, :, c * P:(c + 1) * P])
            S.dma(gb32[d][par][:], gbT[:, c * P:(c + 1) * P])
            S.copy("dve", r(qr[:]), qk[:, 0:8, :])
            S.tr(ps_misc[:, 0:32], gb32[d][par][:], ident[:32, :32])
            g_ = gtm[d][par]
            S.copy("act", g_[:], ps_misc[:, 0:32])
            gcols = g_[:, d * 8:d * 8 + 4]
            bcols = g_[:, 16 + d * 8 + 4:16 + d * 8 + 8]
            S.mm(ps_misc[:, 32:36], self.cst(C_L1(d)), gcols)
            S.mm(ps_misc[:, 36:40], ones, gcols)
            G = gs[d][par]
            S.copy("act", G[:, 0:8], ps_misc[:, 32:40])
            S.act(G[:, 8:16], G[:, 0:8], AF.Exp)
            S.tt("dve", G[:, 16:20], G[:, 4:8], G[:, 0:4], ALU.subtract)
            S.act(G[:, 16:20], G[:, 16:20], AF.Exp)
            S.tt("dve", G[:, 20:24], G[:, 8:12], bcols, ALU.mult)
            for h in range(4):
                pm = nxt("rec", ps_rec)
                S.tr(pm[:, 0:P], qk[:, 4 + h, :], ident)
                S.tr(pm[:, P:2 * P], qk[:, 8 + h, :], ident)
                S.copy("act", ktm[d][par][:, h, :], pm[:, 0:P])
                S.copy("act", vtm[d][par][:, h, :], pm[:, P:2 * P])

        def prep_stages(s, d, h):
            par = s % 2
            qr = qkr[d][par]
            g_ = gtm[d][par]
            G = gs[d][par]
            tb = tmpb[d][h]
            ob = outb[d][h][par]
            kT = qr[:, 4 + h, :]
            qT = qr[:, h, :]
            gcol = g_[:, d * 8 + h:d * 8 + h + 1]
            bcol = g_[:, 16 + d * 8 + 4 + h:16 + d * 8 + 4 + h + 1]
            st = {}
            L = []

            def s0():
                S.act(tb["gU"][:], self.cst(C_U1(d)), AF.Copy, scale=gcol)
                pa = nxt("pa", ps_pa)
                S.mm(pa[:, 0:P], self.cst(C_L1(d)), tb["gU"][:])
                S.mm(pa[:, P:2 * P], tb["gU"][:], self.cst(C_L1(d)))
                S.mm(pa[:, 2 * P:3 * P], r(kT), r(kT))
                S.mm(pa[:, 3 * P:4 * P], r(kT), r(qT))
                S.act(tb["Dm"][:], pa[:, 0:P], AF.Exp)
                S.act(tb["Dt"][:], pa[:, P:2 * P], AF.Exp)
                S.tt("pool", tb["Dm"][:], tb["Dm"][:], self.cst(C_STRICT(d)), ALU.mult)
                S.tt("pool", tb["Dt"][:], tb["Dt"][:], self.cst(C_INCLT(d)), ALU.mult)
                S.stt(tb["A"][:], pa[:, 2 * P:3 * P], bcol, tb["Dm"][:], ALU.mult, ALU.mult)
                S.tt("dve", ob["attnT"][:], pa[:, 3 * P:4 * P], tb["Dt"][:], ALU.mult)
            L.append(s0)

            def s1():
                S.tt("pool", tb["E0f"][:], tb["A"][:], self.cst(C_E(d, 0)), ALU.mult)
                pr = nxt("rec", ps_rec)
                S.tr(pr[:, 0:P], tb["E0f"][:], ident)
                S.tt("dve", r(tb["Xt0"][:]), ident, pr[:, 0:P], ALU.subtract)
                S.tt("pool", tb["X0"][:], ident, tb["E0f"][:], ALU.subtract)
                st["X"], st["Xt"] = tb["X0"], tb["Xt0"]
            L.append(s1)
            for lvl in range(1, 7):
                def sa(lvl=lvl):
                    E = tb["E%d" % (lvl % 2)]
                    S.tt("pool", r(E[:]), tb["A"][:], self.cst(C_E(d, lvl)), ALU.mult)
                    pr = nxt("rec", ps_rec)
                    S.mm(pr[:, 0:P], r(E[:]), r(st["Xt"][:]))
                    S.copy("act", r(tb["H"][:]), pr[:, 0:P])

                def sb(lvl=lvl):
                    X, Xt = st["X"], st["Xt"]
                    pr2 = nxt("rec", ps_rec)
                    S.mm(pr2[:, 0:P], r(X[:]), r(tb["H"][:]))
                    if lvl < 6:
                        S.mm(pr2[:, P:2 * P], r(tb["H"][:]), r(X[:]))
                    Xtn = ob["R"] if lvl == 6 else tb["Xt%d" % (lvl % 2)]
                    S.tt("dve", r(Xtn[:]), Xt[:], pr2[:, 0:P], ALU.subtract)
                    if lvl < 6:
                        Xn = tb["X%d" % (lvl % 2)]
                        S.tt("dve", r(Xn[:]), X[:], pr2[:, P:2 * P], ALU.subtract)
                        st["X"] = Xn
                    st["Xt"] = Xtn
                L.append(sa)
                L.append(sb)

            def sf():
                S.act(r(tb["Kbg"][:]), ktm[d][par][:, h, :], AF.Copy, scale=G[:, 20 + h:21 + h])
                pr = nxt("rec", ps_rec)
                S.mm(pr[:, 0:P], r(tb["Kbg"][:]), r(ob["R"][:]))
                S.act(r(ob["WnT"][:]), pr[:, 0:P], AF.Copy, scale=-1.0)
                S.ts("dve", r(ob["Vb"][:]), vtm[d][par][:, h, :], bcol, None, ALU.mult)
                S.act(r(ob["Kdec"][:]), ktm[d][par][:, h, :], AF.Copy, scale=G[:, 16 + h:17 + h])
            L.append(sf)
            return L

        def prep_all(s):
            for d in range(2):
                prep_shared_d(s, d)
            interleave([prep_stages(s, d, h) for h in range(4) for d in range(2)])

        def seq_stages(s, d, h):
            c = order[d][s]
            par = s % 2
            qk = qkr[d][par]
            G = gs[d][par]
            first = (step_of[d][c] < step_of[1 - d][c]) or (step_of[d][c] == step_of[1 - d][c] and d == 0)
            tb = tmpb[d][h]
            ob = outb[d][h][par]
            Sold = Sst[d][h][s % 2]
            Snew = Sst[d][h][(s + 1) % 2]
            Sob = Sbf[d][h][s % 2]
            Snb = Sbf[d][h][(s + 1) % 2]

            def a():
                p1 = nxt("seq", ps_seq)
                S.mm(p1[:, 0:P], r(ob["R"][:]), r(ob["Vb"][:]), start=True, stop=False)
                S.mm(p1[:, 0:P], ob["WnT"][:], Sob[:], start=False, stop=True)
                S.copy("act", r(tb["vn"][:]), p1[:, 0:P])

            def b_():
                p2 = nxt("seq", ps_seq)
                S.mm(p2[:, 0:P], qk[:, h, :], Sob[:])
                S.mm(p2[:, P:2 * P], r(ob["attnT"][:]), r(tb["vn"][:]))
                S.mm(p2[:, 2 * P:3 * P], r(ob["Kdec"][:]), r(tb["vn"][:]))
                S.copy("act", tb["av"][:], p2[:, P:2 * P])
                if first:
                    S.stt(O[:, c, h, :], p2[:, 0:P], G[:, 8 + h:9 + h], tb["av"][:], ALU.mult, ALU.add)
                else:
                    S.stt(tb["ot"][:], p2[:, 0:P], G[:, 8 + h:9 + h], tb["av"][:], ALU.mult, ALU.add)
                    S.tt("pool", O[:, c, h, :], O[:, c, h, :], tb["ot"][:], ALU.add)
                S.stt(Snew[:], Sold[:], G[:, 12 + h:13 + h], p2[:, 2 * P:3 * P], ALU.mult, ALU.add)
                S.copy("act", Snb[:], Snew[:])
                if "dnS" in self.dump:
                    if "dnS" not in self.d:
                        self.scr("dnS", [18, 2, 4, P, P])
                    S.dma(self.d["dnS"][s, d, h], Snew[:])
            return [a, b_]

        def seq_all(s):
            interleave([seq_stages(s, d, h) for h in range(4) for d in range(2)])

        def prep(s, d):
            raise RuntimeError("unused")

        prep_all(0)
        for s in range(NCH):
            lists = [seq_stages(s, d, h) for h in range(4) for d in range(2)]
            if s + 1 < NCH:
                for d in range(2):
                    prep_shared_d(s + 1, d)
                lists = lists + [prep_stages(s + 1, d, h) for h in range(4) for d in range(2)]
            interleave(lists)
        S.dma(self.d["dnO"], O[:].rearrange("p a b c -> p (a b c)"))
        ph.close()

    def phase_dn_post(self):
        S = self.S
        ph = Phase(self, "dnq")
        NCH = T // P
        ident = self.cst(C_ID)
        O = ph.sb("O", [P, NCH, 4, P])
        S.dma(O[:].rearrange("p a b c -> p (a b c)"), self.d["dnO"])
        ps_rec = [ph.ps("rec") for _ in range(3)]
        cnt = {"rec": 0}

        def nxt(kind, lst):
            p = lst[cnt[kind] % len(lst)]
            cnt[kind] += 1
            return p
        Of = O[:].rearrange("p a b c -> p (a b) c")
        sqt = ph.sb("sqt", [P, NCH * 4, P])
        ms = ph.sb("ms", [P, NCH * 4])
        S.tt("dve", sqt[:], Of, Of, ALU.mult)
        S.reduce(ms[:], sqt[:], AX.X, ALU.add)
        S.ts("dve", ms[:], ms[:], 1.0 / 128.0, RMS_EPS, ALU.mult, ALU.add)
        S.act(ms[:], ms[:], AF.Sqrt)
        S.recip(ms[:], ms[:])
        S.tt("dve", Of, Of, ms[:].unsqueeze(2).to_broadcast([P, NCH * 4, P]), ALU.mult)
        zt = [ph.sb("zt", [P, T]) for _ in range(2)]
        mx = [ph.sb("mx", [P, T], BF16) for _ in range(2)]
        tmpf = [ph.sb("tmpf", [P, P]) for _ in range(2)]
        k = 0
        for h in range(4):
            z_, m_ = zt[h % 2], mx[h % 2]
            S.dma(z_[:], self.d["pT"][(12 + h) * 128:(13 + h) * 128, :])
            S.act(z_[:], z_[:], AF.Silu)
            for c in range(NCH):
                pm = nxt("rec", ps_rec)
                S.tr(pm[:, 0:P], O[:, c, h, :], ident)
                tf = tmpf[k % 2]
                k += 1
                S.act(tf[:], pm[:, 0:P], AF.Copy, scale=self.vcol("dn_norm_g", 0))
                S.tt("dve", m_[:, c * P:(c + 1) * P], tf[:], z_[:, c * P:(c + 1) * P], ALU.mult)
            S.dma(self.d["mixT"][h * 128:(h + 1) * 128, :], m_[:])
        ph.close()


    def phase_hy_prep(self):
        S = self.S
        ph = Phase(self, "hyp")
        pT = self.d["pT"]
        ident = self.cst(C_ID)
        segs = [(0, TL), (TL, TC)]
        NCH = T // P
        xin = [ph.sb("xin", [P, T]) for _ in range(3)]
        cv = [ph.sb("cv", [P, T]) for _ in range(3)]
        vxtm = ph.sb("vxtm", [P, NCH, 512], BF16)
        pss = [ph.ps("ps") for _ in range(2)]
        ip = 0
        for cc in range(4):
            for j in range(3):
                m = 16 + j * 4 + cc
                S.dma(xin[j][:], pT[m * 128:(m + 1) * 128, :])
                ci = j * 4 + cc
                self.conv_fm(cv[j], xin[j], lambda i, ci=ci: self.vcol("hy_conv_w", i * 12 + ci), 3, 1, segs,
                             bias=self.vcol("hy_conv_b", ci))
            S.tt("pool", cv[2][:], cv[2][:], cv[1][:], ALU.mult)
            S.dma(self.d["hyX0"][cc * 128:(cc + 1) * 128, :], cv[0][:])
            S.dma(self.d["hyVX"][cc * 128:(cc + 1) * 128, :], cv[2][:])
            for tc in range(NCH):
                p = pss[ip % 2]
                ip += 1
                S.tr(p[:, 0:P], cv[2][:, tc * P:(tc + 1) * P], ident)
                S.copy("act", vxtm[:, tc, cc * 128:(cc + 1) * 128], p[:, 0:P])
        S.dma(self.d["hyVXtm"].rearrange("(tc p) c -> p tc c", p=P), vxtm[:])
        ph.close()

    def _sin_rr(self, out, x, tmp, n):
        S = self.S
        MAG = 12582912.0
        S.ts("dve", tmp[:, :n], x[:, :n], 1.0 / (2.0 * math.pi), MAG, ALU.mult, ALU.add)
        S.ts("dve", tmp[:, :n], tmp[:, :n], MAG, None, ALU.subtract)
        S.stt(x[:, :n], tmp[:, :n], -2.0 * math.pi, x[:, :n], ALU.mult, ALU.add)
        S.ts("dve", x[:, :n], x[:, :n], -math.pi, math.pi, ALU.max, ALU.min)
        S.act(out[:, :n], x[:, :n], AF.Sin)

    def phase_hy_main(self):
        S = self.S
        ph = Phase(self, "hym")
        NCH = T // P
        hm = VEC_OFF["hy_mlp"]
        b_in, b_m0, b_m1, freq = (self.vec[:64, hm + i:hm + i + 1] for i in range(4))
        w_in = ph.sb("w_in", [33, 64])
        w_mid = ph.sb("w_mid", [64, 2, 64])
        w_out = ph.sb("w_out", [64, 1024])
        S.dma(w_in[:], self.d["hy_w_in"])
        S.dma(w_mid[:], self.d["hy_w_mid"].rearrange("i k j -> k i j"))
        S.dma(w_out[:], self.d["hy_w_out"])
        negt = ph.sb("negt", [P, 18])
        fsc = ph.sb("fsc", [P, 18])
        delt = ph.sb("delt", [P, 512])
        altc = ph.sb("altc", [P, 1], BF16)
        altr = ph.sb("altr", [1, TL], BF16)
        S.dma(negt[:], self.d["hy_negt"])
        S.dma(fsc[:], self.d["hy_fsc"])
        S.dma(delt[:], self.d["hy_delt"])
        S.dma(altc[:], self.d["hy_altc"])
        S.dma(altr[:], self.d["hy_altr"])
        vxtm = ph.sb("vxtm", [P, NCH, 512], BF16)
        S.dma(vxtm[:], self.d["hyVXtm"].rearrange("(tc p) c -> p tc c", p=P))
        hs = ph.sb("hs", [P, NCH, 512], BF16)
        hd = ph.sb("hd", [P, NCH, 512], BF16)
        Y = ph.sb("Y", [P, NCH, 2, 512], BF16)
        YL = ph.sb("YL", [1, 2, 512], BF16)
        pss = [ph.ps("ps") for _ in range(8)]
        ip = [0]

        def nps():
            p = pss[ip[0] % 8]
            ip[0] += 1
            return p

        cfgs = [(TL, "hy_zT_lat", 0, 0, "dftC_lat", "dftS_lat"), (TC, "hy_zT_ctx", 16, TL, "dftC_ctx", "dftS_ctx")]
        pa_ = Phase(self, "hymA")
        zt = pa_.sb("zt", [33, TL])
        ha = pa_.sb("ha", [64, TL])
        hb_ = pa_.sb("hb", [64, TL])
        xa = pa_.sb("xa", [64, 512])
        xb = pa_.sb("xb", [64, 512])
        dec = [pa_.sb("dec", [P, 512]) for _ in range(2)]
        hf32 = [pa_.sb("hf32", [P, 512]) for _ in range(2)]
        hb32 = [pa_.sb("hb32", [P, 512]) for _ in range(2)]
        for (L, zname, tcb, tok0, cn, sn) in cfgs:
            S.dma(zt[:, :L], self.d[zname])
            layers = [(w_in[:, :], zt, b_in, ha), (w_mid[:, 0, :], ha, b_m0, hb_), (w_mid[:, 1, :], hb_, b_m1, ha)]
            for (wl, src, bl, dst) in layers:
                for t0 in range(0, L, 512):
                    n = min(512, L - t0)
                    p = nps()
                    S.mm(p[:64, :n], wl, src[:, t0:t0 + n])
                    S.ts("dve", xa[:, :n], p[:64, :n], bl, freq, ALU.add, ALU.mult)
                    self._sin_rr(dst[:, t0:t0 + n], xa, xb, n)
            h3 = ha
            for nc_ in range(L // P):
                k = nc_ % 2
                pf = nps()
                pb = nps()
                S.mm(pf[:, :], h3[:, nc_ * P:(nc_ + 1) * P], w_out[:, 0:512])
                S.mm(pb[:, :], h3[:, nc_ * P:(nc_ + 1) * P], w_out[:, 512:1024])
                S.act(dec[k][:], delt[:], AF.Exp, scale=negt[:, tcb + nc_:tcb + nc_ + 1])
                S.tt("dve", hf32[k][:], pf[:, :], dec[k][:], ALU.mult)
                S.tt("dve", hb32[k][:], pb[:, :], dec[k][:], ALU.mult)
                if nc_ == 0:
                    S.memset("pool", hb32[k][0:1, :], 0.0)
                S.tt("pool", hs[:, tcb + nc_, :], hf32[k][:], hb32[k][:], ALU.add)
                S.tt("pool", hd[:, tcb + nc_, :], hf32[k][:], hb32[k][:], ALU.subtract)
        if "hyH" in self.dump:
            o = self.scr("hyH", [P, NCH * 512], BF16)
            S.dma(o, hs[:].rearrange("p a b -> p (a b)"))
        pa_.close()
        pb_ = Phase(self, "hymB")
        Ct = [pb_.sb("Ct", [P, 16, P], BF16) for _ in range(2)]
        St = [pb_.sb("St", [P, 16, P], BF16) for _ in range(2)]
        b1s = [pb_.sb("b1s", [P, 512]) for _ in range(2)]
        b2s = [pb_.sb("b2s", [P, 512]) for _ in range(2)]
        m1 = [pb_.sb("m1", [P, 512]) for _ in range(2)]
        m2 = [pb_.sb("m2", [P, 512]) for _ in range(2)]
        it = 0
        for (L, zname, tcb, tok0, cn, sn) in cfgs:
            ntc = L // P
            Cd = self.d[cn].rearrange("(tc p) f -> p tc f", p=P)
            Sd = self.d[sn].rearrange("(tc p) f -> p tc f", p=P)
            for fc in range(ntc):
                k = it % 2
                it += 1
                S.dma(Ct[k][:, :ntc, :], Cd[:, :, fc * P:(fc + 1) * P])
                S.dma(St[k][:, :ntc, :], Sd[:, :, fc * P:(fc + 1) * P])
                pA1, pA2, pB1, pB2 = nps(), nps(), nps(), nps()
                for (pp, tab, src) in ((pA1, Ct[k], vxtm), (pA2, St[k], vxtm), (pB1, Ct[k], hs), (pB2, St[k], hd)):
                    for tc in range(ntc):
                        S.mm(pp[:, :], tab[:, tc, :], src[:, tcb + tc, :], start=(tc == 0), stop=(tc == ntc - 1))
                S.copy("act", b1s[k][:], pB1[:, :])
                S.copy("act", b2s[k][:], pB2[:, :])
                fcol = fsc[:, tcb + fc:tcb + fc + 1]
                S.tt("dve", m1[k][:], pA1[:, :], b1s[k][:], ALU.mult)
                S.tt("dve", m2[k][:], pA2[:, :], b2s[k][:], ALU.mult)
                S.tt("pool", m1[k][:], m1[k][:], m2[k][:], ALU.subtract)
                S.act(Y[:, tcb + fc, 0, :], m1[k][:], AF.Copy, scale=fcol)
                S.tt("dve", b2s[k][:], pA1[:, :], b2s[k][:], ALU.mult)
                S.tt("dve", b1s[k][:], pA2[:, :], b1s[k][:], ALU.mult)
                S.tt("pool", b1s[k][:], b1s[k][:], b2s[k][:], ALU.add)
                S.act(Y[:, tcb + fc, 1, :], b1s[k][:], AF.Copy, scale=fcol)
            li = 0 if L == TL else 1
            pU, pK = nps(), nps()
            for tc in range(ntc):
                S.mm(pU[0:1, :], altc[:, 0:1], vxtm[:, tcb + tc, :], start=(tc == 0), stop=(tc == ntc - 1))
            for tc in range(ntc):
                S.mm(pK[0:1, :], altc[:, 0:1], hs[:, tcb + tc, :], start=(tc == 0), stop=(tc == ntc - 1))
            S.copy("act", m1[0][0:1, :], pK[0:1, :])
            S.stt(YL[0:1, li, :], pU[0:1, :], 1.0 / (2.0 * L), m1[0][0:1, :], ALU.mult, ALU.mult)
        pb_.close()
        pc_ = Phase(self, "hymC")
        Cf = [pc_.sb("Cf", [P, 16, 512], BF16) for _ in range(1)]
        Sf = [pc_.sb("Sf", [P, 16, 512], BF16) for _ in range(1)]
        x0t = [pc_.sb("x0t", [P, 512]) for _ in range(2)]
        vxt = [pc_.sb("vxt", [P, 512]) for _ in range(2)]
        ot = [pc_.sb("ot", [P, 512], BF16) for _ in range(2)]
        it = 0
        for (L, zname, tcb, tok0, cn, sn) in cfgs:
            nfc = L // P
            li = 0 if L == TL else 1
            Cd = self.d[cn].rearrange("(fc p) t -> p fc t", p=P)
            Sd = self.d[sn].rearrange("(fc p) t -> p fc t", p=P)
            for t0 in range(0, L, 512):
                n = min(512, L - t0)
                S.dma(Cf[0][:, :nfc, :n], Cd[:, :, t0:t0 + n])
                S.dma(Sf[0][:, :nfc, :n], Sd[:, :, t0:t0 + n])
                for cc in range(4):
                    k = it % 2
                    it += 1
                    S.dma(x0t[k][:, :n], self.d["hyX0"][cc * 128:(cc + 1) * 128, tok0 + t0:tok0 + t0 + n])
                    S.dma(vxt[k][:, :n], self.d["hyVX"][cc * 128:(cc + 1) * 128, tok0 + t0:tok0 + t0 + n])
                    p = nps()
                    for fc in range(nfc):
                        S.mm(p[:, :n], Y[:, tcb + fc, 0, cc * 128:(cc + 1) * 128], Cf[0][:, fc, :n], start=(fc == 0), stop=False)
                        S.mm(p[:, :n], Y[:, tcb + fc, 1, cc * 128:(cc + 1) * 128], Sf[0][:, fc, :n], start=False, stop=False)
                    S.mm(p[:, :n], YL[0:1, li, cc * 128:(cc + 1) * 128], altr[0:1, t0:t0 + n], start=False, stop=True)
                    S.stt(vxt[k][:, :n], vxt[k][:, :n], self.vcol("hy_skip", cc), p[:, :n], ALU.mult, ALU.add)
                    S.tt("pool", ot[k][:, :n], vxt[k][:, :n], x0t[k][:, :n], ALU.mult)
                    S.dma(self.d["mixT"][512 + cc * 128:512 + (cc + 1) * 128, tok0 + t0:tok0 + t0 + n], ot[k][:, :n])
        pc_.close()
        ph.close()


    def phase_post(self, l):
        S = self.S
        ph = Phase(self, "post%d" % l)
        ident = self.cst(C_ID)
        ones = self.cst(C_ONES)
        xsrc = self.d["xT0"] if l == 0 else self.d["xT1"]
        wsrc = self.d["ev_w_out"] if l == 0 else self.d["od_w_out"]
        wsb = ph.sb("w", [P, 8, D], BF16)
        S.dma(wsb[:], wsrc.rearrange("(kc p) n -> p kc n", p=P), eng="pool")
        rw = ph.sb("rw", [P, 8, NE])
        S.dma(rw[:], self.d["router_w"][l].rearrange("(kc p) e -> p kc e", p=P))
        rb = ph.sb("rb", [P, NE])
        S.dma(rb[:], self.d["router_b"][l].partition_broadcast(P))
        gbc = ph.sb("gbc", [P, 8, 2])
        for kc in range(8):
            for w in range(2):
                if l == 1:
                    S.tt("pool", gbc[:, kc, w:w + 1], self.m_g1(l, kc, w), self.vcol("od_b_out", kc), ALU.mult)
                else:
                    S.memset("pool", gbc[:, kc, w:w + 1], 0.0)
        mixs = [ph.sb("mix", [P, 8, 512], BF16) for _ in range(2)]
        xts = [ph.sb("xt", [P, 8, 512]) for _ in range(2)]
        zs = [ph.sb("z", [P, 8, 512]) for _ in range(2)]
        zsq = [ph.sb("zsq", [P, 512]) for _ in range(2)]
        x1s = [ph.sb("x1", [P, 8, 512]) for _ in range(2)]
        v32s = [ph.sb("v32", [P, 8, 512]) for _ in range(2)]
        vbs = [ph.sb("vb", [P, 8, 512], BF16)] * 2
        means = [ph.sb("mean", [P, 512])] * 2
        rstds = [ph.sb("rstd", [P, 512])] * 2
        lg = [ph.sb("lg", [P, NE]) for _ in range(2)]
        ex = [ph.sb("ex", [P, NE]) for _ in range(2)]
        mx = [ph.sb("mx", [P, 8]) for _ in range(2)]
        sm = [ph.sb("sm", [P, 4]) for _ in range(2)]
        gTs = [ph.sb("gT", [NE, 512]) for _ in range(2)]
        ps_y = [ph.ps("psy") for _ in range(2)]
        ps_s1s = [ph.ps("s1") for _ in range(2)]
        ps_s2s = [ph.ps("s2") for _ in range(2)]
        ps_lg = ph.ps("lg")
        ps_tr = ph.ps("tr")
        tiles = ALL_TILES if l == 0 else ALL_TILES[:4]
        cnt = {"iy": 0, "ir": 0}

        def stage_a(ti):
            t0, n, w = tiles[ti]
            mix, xt, z, x1, v32, vb = mixs[ti % 2], xts[ti % 2], zs[ti % 2], x1s[ti % 2], v32s[ti % 2], vbs[ti % 2]
            ps_s1, ps_s2 = ps_s1s[ti % 2], ps_s2s[ti % 2]
            S.dma(mix[:, :, :n], self.d["mixT"].rearrange("(kc p) t -> p kc t", p=P)[:, :, t0:t0 + n])
            S.dma(xt[:, :, :n], xsrc.rearrange("(kc p) t -> p kc t", p=P)[:, :, t0:t0 + n])
            for dc in range(8):
                p = ps_y[cnt["iy"] % 2]
                cnt["iy"] += 1
                for kc in range(8):
                    S.mm(p[:, :n], wsb[:, kc, dc * 128:(dc + 1) * 128], mix[:, kc, :n], start=(kc == 0), stop=(kc == 7))
                S.act(xt[:, dc, :n], xt[:, dc, :n], AF.Identity, scale=ALPHA, bias=gbc[:, dc, w:w + 1])
                S.stt(z[:, dc, :n], p[:, :n], self.m_g1(l, dc, w), xt[:, dc, :n], ALU.mult, ALU.add)
                q = zsq[dc % 2]
                S.act(q[:, :n], z[:, dc, :n], AF.Square)
                S.mm(ps_s1[:, :n], ones, z[:, dc, :n], start=(dc == 0), stop=(dc == 7))
                S.mm(ps_s2[:, :n], ones, q[:, :n], start=(dc == 0), stop=(dc == 7))
            self._ln_apply(l, 0, w, n, z, ps_s1, ps_s2, means[ti % 2], rstds[ti % 2], x1)
            S.dma(self.d["x1T"].rearrange("(kc p) t -> p kc t", p=P)[:, :, t0:t0 + n], x1[:, :, :n])
            for dc in range(8):
                S.ts("dve", v32[:, dc, :n], x1[:, dc, :n], self.m_a2(l, dc, w), self.m_sh2(l, dc, w), ALU.mult, ALU.add)
                S.copy("act", vb[:, dc, :n], v32[:, dc, :n])
            S.dma(self.d["hT"].rearrange("(kc p) t -> p kc t", p=P)[:, :, t0:t0 + n], vb[:, :, :n])

        def stage_b(ti):
            t0, n, w = tiles[ti]
            v32, gT = v32s[ti % 2], gTs[ti % 2]
            for c0 in range(0, n, P):
                k = cnt["ir"] % 2
                cnt["ir"] += 1
                for kc in range(8):
                    S.mm(ps_lg[:, 0:NE], v32[:, kc, c0:c0 + P], rw[:, kc, :], start=(kc == 0), stop=(kc == 7))
                S.tt("dve", lg[k][:], ps_lg[:, 0:NE], rb[:], ALU.add)
                S.max8(mx[k][:], lg[k][:])
                S.ts("dve", sm[k][:, 0:1], mx[k][:, 0:1], -1.0, None, ALU.mult)
                S.act(ex[k][:], lg[k][:], AF.Exp, bias=sm[k][:, 0:1])
                S.ts("dve", lg[k][:], lg[k][:], mx[k][:, 3:4], None, ALU.is_ge)
                S.tt("dve", ex[k][:], ex[k][:], lg[k][:], ALU.mult)
                S.reduce(sm[k][:, 1:2], ex[k][:], AX.X, ALU.add)
                S.recip(sm[k][:, 2:3], sm[k][:, 1:2])
                S.ts("dve", ex[k][:], ex[k][:], sm[k][:, 2:3], None, ALU.mult)
                S.tr(ps_tr[:NE, 0:P], ex[k][:], ident)
                S.copy("act", gT[:, c0:c0 + P], ps_tr[:NE, 0:P])
            S.dma(self.d["gateT"][:, t0:t0 + n], gT[:, :n])

        stage_a(0)
        for ti in range(len(tiles)):
            if ti + 1 < len(tiles):
                stage_a(ti + 1)
            stage_b(ti)
        ph.close()

    def _ln_apply(self, l, which, w, n, z, ps_s1, ps_s2, mean, rstd, out):
        S = self.S
        S.act(mean[:, :n], ps_s1[:, :n], AF.Copy, scale=1.0 / D)
        S.tt("pool", rstd[:, :n], mean[:, :n], mean[:, :n], ALU.mult)
        S.stt(rstd[:, :n], ps_s2[:, :n], 1.0 / D, rstd[:, :n], ALU.mult, ALU.subtract)
        S.ts("dve", rstd[:, :n], rstd[:, :n], LN_EPS, None, ALU.add)
        S.act(rstd[:, :n], rstd[:, :n], AF.Sqrt)
        S.recip(rstd[:, :n], rstd[:, :n])
        for dc in range(8):
            e = "pool" if dc % 2 else "dve"
            S.tt(e, z[:, dc, :n], z[:, dc, :n], mean[:, :n], ALU.subtract)
            S.tt(e, z[:, dc, :n], z[:, dc, :n], rstd[:, :n], ALU.mult)
            S.act(out[:, dc, :n], z[:, dc, :n], AF.Identity, scale=self.vcol("ln_g", (l * 2 + which) * 8 + dc),
                  bias=self.vcol("ln_b", (l * 2 + which) * 8 + dc))


    def phase_moe(self, l):
        S = self.S
        ph = Phase(self, "moe%d" % l)
        ones = self.cst(C_ONES)
        last = (l == 1)
        if not last:
            ttiles = [(0, [384, 384], [0, 0]), (768, [384, 384], [0, 0]), (1536, [384, 128, 256], [0, 0, 1])]
        else:
            ttiles = [(0, [384, 384], [0, 0]), (768, [384, 384], [0, 0]), (1536, [256, 256], [0, 0])]
        TT = 768
        xdst = self.d["out"] if last else self.d["xT1"]
        w1src = self.d["moe_w1r"]
        w2src = self.d["moe_w2r"]
        b2sb = ph.sb("b2", [NE, D])
        S.dma(b2sb[:], self.d["moe_b2"][l])
        hT = ph.sb("hT", [P, 8, TT], BF16)
        gT = ph.sb("gT", [NE, TT])
        acts = [ph.sb("act", [P, 8, TT], BF16) for _ in range(2)]
        yacc = ph.sb("yacc", [P, 8, TT])
        w1b = [ph.sb("w1", [P, 8, 1024], BF16) for _ in range(3)]
        w2b = [ph.sb("w2", [P, 8, 1024], BF16) for _ in range(2)]
        gb = [ph.sb("gb", [P, TT]) for _ in range(2)]
        NTMP = 5
        tg = [ph.sb("tg", [P, 384]) for _ in range(NTMP)]
        tsg = [ph.sb("ts", [P, 384]) for _ in range(NTMP)]
        tl = [ph.sb("tl", [P, 384]) for _ in range(NTMP)]
        mean, rstd = tg[0], tg[1]
        x1r = [tsg[0], tsg[1]]
        zsq = [tl[0], tl[1]]
        pipe = []

        def pipe_step(new):
            if new is not None:
                pipe.append(new)
                it = new
                S.ts("dve", it["g"], it["pg"], it["bg"], 7.0, ALU.add, ALU.min)
                S.act(it["l"], it["pl"], AF.Identity, bias=it["bl"])
            else:
                pipe.append(None)
            if len(pipe) >= 2 and pipe[-2] is not None:
                it = pipe[-2]
                S.act(it["s"], it["g"], AF.Sigmoid, scale=1.702)
                S.ts("dve", it["l"], it["l"], 8.0, -6.0, ALU.min, ALU.max)
                S.tt("pool", it["g"], it["g"], it["gate"], ALU.mult)
            if len(pipe) >= 3 and pipe[-3] is not None:
                it = pipe[-3]
                S.tt("pool", it["l"], it["l"], it["g"], ALU.mult)
            if len(pipe) >= 4 and pipe[-4] is not None:
                it = pipe[-4]
                S.tt("dve", it["out"], it["l"], it["s"], ALU.mult)
            if len(pipe) > 4:
                pipe.pop(0)

        def pipe_flush():
            for _ in range(3):
                pipe_step(None)
            del pipe[:]
        ps_g = [ph.ps("psg") for _ in range(2)]
        ps_l = [ph.ps("psl") for _ in range(2)]
        ps_y = [ph.ps("psy") for _ in range(2)]
        ps_s1 = ph.ps("s1")
        ps_s2 = ph.ps("s2")
        cnt = {"w1": 0, "e": 0, "gl": 0, "y": 0, "tmp": 0}
        b1o = VEC_OFF["moe_b1"]

        NEr = self.cfg.get("moe_experts", NE)
        for (tt0, subs, ws) in ttiles:
            nt = sum(subs)
            offs = [sum(subs[:i]) for i in range(len(subs))]
            S.dma(hT[:, :, :nt], self.d["hT"].rearrange("(kc p) t -> p kc t", p=P)[:, :, tt0:tt0 + nt])
            S.dma(gT[:, :nt], self.d["gateT"][:, tt0:tt0 + nt])
            for dc in range(8):
                for (o, n) in zip(offs, subs):
                    p = ps_y[cnt["y"] % 2]
                    cnt["y"] += 1
                    S.mm(p[:, :n], b2sb[:, dc * 128:(dc + 1) * 128], gT[:, o:o + n])
                    S.copy("act", yacc[:, dc, o:o + n], p[:, :n])

            wslot = {}

            def dma_w1(e, jh):
                if e >= NEr:
                    return
                wt = w1b[(2 * e + jh) % 3]
                S.dma(wt[:], w1src[l, e, jh], eng="pool")
                wslot[(e, jh)] = wt

            def dma_w2(e):
                if e >= NEr:
                    return
                S.dma(w2b[e % 2][:], w2src[l, e], eng="pool")

            def stage1(e):
                k = e % 2
                act = acts[k]
                g_b = gb[k]
                S.dma(g_b[:, :nt], self.d["gateT"][e:e + 1, tt0:tt0 + nt].partition_broadcast(P))
                dma_w1(e + 1, 0)
                dma_w2(e)
                wh = [wslot[(e, 0)], wslot[(e, 1)]]
                for j in range(8):
                    if j == 5:
                        dma_w1(e + 1, 1)
                    wt = wh[j // 4]
                    c0 = (j % 4) * 256
                    bg = self.vec[:, b1o + (l * 32 + e) * 16 + j:b1o + (l * 32 + e) * 16 + j + 1]
                    bl = self.vec[:, b1o + (l * 32 + e) * 16 + 8 + j:b1o + (l * 32 + e) * 16 + 8 + j + 1]
                    for (o, n) in zip(offs, subs):
                        pg = ps_g[cnt["gl"] % 2]
                        pl = ps_l[cnt["gl"] % 2]
                        cnt["gl"] += 1
                        for kc in range(8):
                            S.mm(pg[:, :n], wt[:, kc, c0:c0 + 128], hT[:, kc, o:o + n], start=(kc == 0), stop=(kc == 7))
                        for kc in range(8):
                            S.mm(pl[:, :n], wt[:, kc, c0 + 128:c0 + 256], hT[:, kc, o:o + n], start=(kc == 0), stop=(kc == 7))
                        q = cnt["tmp"] % NTMP
                        cnt["tmp"] += 1
                        pipe_step({"g": tg[q][:, :n], "s": tsg[q][:, :n], "l": tl[q][:, :n], "pg": pg[:, :n], "pl": pl[:, :n],
                                   "bg": bg, "bl": bl, "gate": g_b[:, o:o + n], "out": act[:, j, o:o + n]})

            def stage2(e):
                k = e % 2
                act = acts[k]
                wt = w2b[k]
                for dc in range(8):
                    for (o, n) in zip(offs, subs):
                        p = ps_y[cnt["y"] % 2]
                        cnt["y"] += 1
                        for fc in range(8):
                            S.mm(p[:, :n], wt[:, fc, dc * 128:(dc + 1) * 128], act[:, fc, o:o + n], start=(fc == 0), stop=(fc == 7))
                        S.tt("dve", yacc[:, dc, o:o + n], yacc[:, dc, o:o + n], p[:, :n], ALU.add)

            dma_w1(0, 0)
            dma_w1(0, 1)
            stage1(0)
            for e in range(NEr):
                if e + 1 < NEr:
                    stage1(e + 1)
                else:
                    pipe_flush()
                stage2(e)
            if "fT" in self.dump:
                if "fT" not in self.d:
                    self.scr("fT", [D, T])
                S.dma(self.d["fT"].rearrange("(kc p) t -> p kc t", p=P)[:, :, tt0:tt0 + nt], yacc[:, :, :nt])
            x1v = self.d["x1T"].rearrange("(kc p) t -> p kc t", p=P)
            xdv = xdst.rearrange("(kc p) t -> p kc t", p=P)
            for (o, n, w) in zip(offs, subs, ws):
                for dc in range(8):
                    xr = x1r[dc % 2]
                    S.dma(xr[:, :n], x1v[:, dc, tt0 + o:tt0 + o + n])
                    S.act(xr[:, :n], xr[:, :n], AF.Copy, scale=ALPHA)
                    S.stt(yacc[:, dc, o:o + n], yacc[:, dc, o:o + n], self.m_g2(l, dc, w), xr[:, :n], ALU.mult, ALU.add)
                    q = zsq[dc % 2]
                    S.act(q[:, :n], yacc[:, dc, o:o + n], AF.Square)
                    S.mm(ps_s1[:, :n], ones, yacc[:, dc, o:o + n], start=(dc == 0), stop=(dc == 7))
                    S.mm(ps_s2[:, :n], ones, q[:, :n], start=(dc == 0), stop=(dc == 7))
                zv = yacc[:, :, o:o + n]
                self._ln_apply2(l, 1, n, yacc, o, ps_s1, ps_s2, mean, rstd)
                S.dma(xdv[:, :, tt0 + o:tt0 + o + n], yacc[:, :, o:o + n])
        ph.close()

    def _ln_apply2(self, l, which, n, z, o, ps_s1, ps_s2, mean, rstd):
        S = self.S
        S.act(mean[:, :n], ps_s1[:, :n], AF.Copy, scale=1.0 / D)
        S.tt("pool", rstd[:, :n], mean[:, :n], mean[:, :n], ALU.mult)
        S.stt(rstd[:, :n], ps_s2[:, :n], 1.0 / D, rstd[:, :n], ALU.mult, ALU.subtract)
        S.ts("dve", rstd[:, :n], rstd[:, :n], LN_EPS, None, ALU.add)
        S.act(rstd[:, :n], rstd[:, :n], AF.Sqrt)
        S.recip(rstd[:, :n], rstd[:, :n])
        for dc in range(8):
            e = "pool" if dc % 2 else "dve"
            zz = z[:, dc, o:o + n]
            S.tt(e, zz, zz, mean[:, :n], ALU.subtract)
            S.tt(e, zz, zz, rstd[:, :n], ALU.mult)
            S.act(zz, zz, AF.Identity, scale=self.vcol("ln_g", (l * 2 + which) * 8 + dc),
                  bias=self.vcol("ln_b", (l * 2 + which) * 8 + dc))


    def phase_l1_proj(self):
        S = self.S
        ph = Phase(self, "l1a")
        wsb = ph.sb("w", [P, 8, 2048], BF16)
        src = self.d["od_w_in"].rearrange("(kc p) n -> p kc n", p=P)
        for q in range(2):
            S.dma(wsb[:, 4 * q:4 * q + 4, :], src[:, 4 * q:4 * q + 4, :], eng="pool")
        xts = [ph.sb("xt", [P, 8, 512]) for _ in range(2)]
        pts = [ph.sb("pt", [P, 8, 512]) for _ in range(2)]
        us = [ph.sb("u", [P, 8, 512], BF16) for _ in range(2)]
        stg = [ph.sb("stg", [P, 4, 512]) for _ in range(3)]
        pss = [ph.ps("ps") for _ in range(4)]
        ip = 0
        ist = 0
        for ti, (t0, n, w) in enumerate(ALL_TILES):
            xt, pt, u = xts[ti % 2], pts[ti % 2], us[ti % 2]
            self.make_u(ph, 1, self.d["xT1"], t0, n, w, xt, pt, u)
            for m0 in range(0, 16, 4):
                sg = stg[ist % 3]
                ist += 1
                for m in range(m0, m0 + 4):
                    p = pss[ip % 4]
                    ip += 1
                    for kc in range(8):
                        S.mm(p[:, :n], wsb[:, kc, m * 128:(m + 1) * 128], u[:, kc, :n], start=(kc == 0), stop=(kc == 7))
                    if m < 8:
                        S.ts("dve", sg[:, m - m0, :n], p[:, :n], self.vcol("od_b_in", m), None, ALU.add)
                    else:
                        S.act(sg[:, m - m0, :n], p[:, :n], AF.Gelu, bias=self.vcol("od_b_in", m))
                dst = self.d["lruP"][m0 * 128:(m0 + 4) * 128, t0:t0 + n].rearrange("(m p) t -> p m t", p=P)
                S.dma(dst, sg[:, :, :n])
        ph.close()

    def phase_lru(self):
        S = self.S
        ph = Phase(self, "lru")
        segs = [(0, TL), (TL, TC)]
        lruP = self.d["lruP"]
        wa = ph.sb("wa", [P, 8, 2, 256], BF16)
        wx = ph.sb("wx", [P, 8, 2, 256], BF16)
        S.dma(wa[:], self.d["lru_wa"].rearrange("d h (ic p) j -> p (d h) ic j", p=P), eng="pool")
        S.dma(wx[:], self.d["lru_wx"].rearrange("d h (ic p) j -> p (d h) ic j", p=P), eng="pool")
        nsp = ph.sb("nsp", [P, 16])
        ao = VEC_OFF["lru_a"]
        S.act(nsp[:], self.vec[:, ao:ao + 16], AF.Exp)
        S.act(nsp[:], nsp[:], AF.Ln, bias=self.c_one)
        S.ts("dve", nsp[:], nsp[:], -8.0, None, ALU.mult)
        xcb = ph.sb("xcb", [P, 8, T], BF16)
        xin = [ph.sb("xin", [P, T]) for _ in range(2)]
        xc = [ph.sb("xc", [P, T]) for _ in range(2)]
        for c in range(8):
            a, o = xin[c % 2], xc[c % 2]
            S.dma(a[:], lruP[c * 128:(c + 1) * 128, :])
            self.conv_fm(o, a, lambda i, c=c: self.vcol("lru_conv_w", i * 8 + c), 4, 2, segs, bias=self.vcol("lru_conv_b", c))
            S.dma(self.d["lruXC"][c * 128:(c + 1) * 128, :], o[:])
            S.copy("act", xcb[:, c, :], o[:])
        r_ = ph.sb("r", [P, T])
        i_ = ph.sb("i", [P, T])
        a_ = ph.sb("a", [P, T])
        b_ = ph.sb("b", [P, T])
        h_ = ph.sb("h", [P, T])
        hs_ = ph.sb("hs", [P, T])
        gel = ph.sb("gel", [P, TL])
        ob = ph.sb("ob", [P, TL], BF16)
        pss = [ph.ps("ps") for _ in range(4)]
        ip = 0
        for c in range(8):
            hh = c // 2
            x32 = xc[c % 2]
            S.dma(x32[:], self.d["lruXC"][c * 128:(c + 1) * 128, :])
            S.dma(gel[:], lruP[(8 + c) * 128:(9 + c) * 128, 0:TL])
            for d in range(2):
                for (t0, n, w) in ALL_TILES:
                    pr = pss[ip % 4]
                    pi = pss[(ip + 1) % 4]
                    ip += 2
                    for ic in range(2):
                        S.mm(pr[:, :n], wa[:, d * 4 + hh, ic, (c % 2) * 128:(c % 2) * 128 + 128], xcb[:, 2 * hh + ic, t0:t0 + n],
                             start=(ic == 0), stop=(ic == 1))
                    for ic in range(2):
                        S.mm(pi[:, :n], wx[:, d * 4 + hh, ic, (c % 2) * 128:(c % 2) * 128 + 128], xcb[:, 2 * hh + ic, t0:t0 + n],
                             start=(ic == 0), stop=(ic == 1))
                    S.act(r_[:, t0:t0 + n], pr[:, :n], AF.Sigmoid, bias=self.vcol("lru_ba", d * 8 + c))
                    S.act(i_[:, t0:t0 + n], pi[:, :n], AF.Sigmoid, bias=self.vcol("lru_bx", d * 8 + c))
                S.act(a_[:], r_[:], AF.Exp, scale=nsp[:, d * 8 + c:d * 8 + c + 1])
                S.tt("pool", b_[:], a_[:], a_[:], ALU.mult)
                S.act(b_[:], b_[:], AF.Sqrt, scale=-1.0, bias=self.c_one)
                S.tt("pool", b_[:], b_[:], i_[:], ALU.mult)
                S.tt("dve", b_[:], b_[:], x32[:], ALU.mult)
                if d == 0:
                    S.scan(h_[:, TL:T], a_[:, TL:T], b_[:, TL:T], 0.0, ALU.mult, ALU.add)
                    S.scan(hs_[:, 0:TL], a_[:, 0:TL], b_[:, 0:TL], h_[:, T - 1:T], ALU.mult, ALU.add)
                else:
                    S.scan(h_[:, T - 1:TL - 1:-1], a_[:, T - 1:TL - 1:-1], b_[:, T - 1:TL - 1:-1], 0.0, ALU.mult, ALU.add)
                    S.scan(h_[:, TL - 1::-1], a_[:, TL - 1::-1], b_[:, TL - 1::-1], h_[:, TL:TL + 1], ALU.mult, ALU.add)
                    S.tt("pool", hs_[:, 0:TL], hs_[:, 0:TL], h_[:, 0:TL], ALU.add)
            S.tt("dve", ob[:], hs_[:, 0:TL], gel[:], ALU.mult)
            S.dma(self.d["mixT"][c * 128:(c + 1) * 128, 0:TL], ob[:])
        ph.close()

    def declare(self):
        cfg = self.cfg
        self.inp("vec", [P, NV])
        self.inp("consts", [P, NCONST, P])
        self.inp("cc", [P, 16])
        self.inp("ada_w", [2, D, 6 * D])
        self.inp("ada_b_raw", [2, 6 * D])
        self.inp("xT0", [D, T])
        self.inp("posT", [D, TL])
        self.inp("ev_w_in", [D, 3600])
        self.scr("pT", [3600, T])
        self.inp("gparams", [16, 2])
        self.scr("qkvT", [1536, T])
        self.scr("gbT", [32, T])
        self.scr("dnO", [P, 18 * 4 * P])
        self.scr("mixT", [D, T], BF16)
        self.scr("hyX0", [512, T])
        self.inp("ev_w_out", [D, D])
        self.inp("od_w_out", [D, D])
        self.inp("router_w", [2, D, NE])
        self.inp("router_b", [2, NE])
        self.scr("x1T", [D, T])
        if self.cfg.get("xT1_input"):
            self.inp("xT1", [D, T])
        else:
            self.scr("xT1", [D, T])
        self.inp("od_w_in", [D, 2048])
        self.inp("lru_wa", [2, 4, 256, 256])
        self.inp("lru_wx", [2, 4, 256, 256])
        self.scr("lruP", [2048, T])
        self.scr("lruXC", [D, T])
        self.scr("hT", [D, T], BF16)
        self.scr("gateT", [NE, T])
        if self.cfg.get("moe", True):
            wl, we = self.cfg.get("moe_wshape", (2, NE))
            self.inp("moe_w1r", [wl, we, 2, P, 8, 1024])
            self.inp("moe_w2r", [wl, we, P, 8, 1024])
            self.inp("moe_b2", [2, NE, D])
        self.scr("out", [D, TL], out=True)
        self.scr("hyVX", [512, T])
        self.scr("hyVXtm", [T, 512], BF16)
        self.inp("hy_w_in", [33, 64])
        self.inp("hy_w_mid", [2, 64, 64])
        self.inp("hy_w_out", [64, 1024])
        self.inp("hy_negt", [P, 18])
        self.inp("hy_fsc", [P, 18])
        self.inp("hy_delt", [P, 512])
        self.inp("hy_altc", [P, 1], BF16)
        self.inp("hy_altr", [1, TL], BF16)
        self.inp("hy_zT_lat", [33, TL])
        self.inp("hy_zT_ctx", [33, TC])
        self.inp("dftC_lat", [TL, TL], BF16)
        self.inp("dftS_lat", [TL, TL], BF16)
        self.inp("dftC_ctx", [TC, TC], BF16)
        self.inp("dftS_ctx", [TC, TC], BF16)


def build_program(cfg):
    nc = bass.Bass("TRN2", target_bir_lowering=False)
    stack = contextlib.ExitStack()
    K = KB(nc, stack, cfg)
    K.declare()
    K.setup()
    for phn in cfg["phases"]:
        if isinstance(phn, tuple):
            getattr(K, "phase_" + phn[0])(*phn[1:])
        else:
            getattr(K, "phase_" + phn)()
    K.S.barrier()
    stack.close()
    return nc, K


_CONST_CACHE = {}


def host_consts():
    if not _CONST_CACHE:
        _CONST_CACHE["consts"] = make_consts()
        _CONST_CACHE["posT"] = make_pos()
        zl, tl = make_hy_z(TL)
        zc, tcx = make_hy_z(TC)
        _CONST_CACHE["hy_zT_lat"] = zl
        _CONST_CACHE["hy_zT_ctx"] = zc
        negt = np.zeros((P, 18), np.float32)
        negt[:, 0:16] = -tl.reshape(16, P).T
        negt[:, 16:18] = -tcx.reshape(2, P).T
        _CONST_CACHE["hy_negt"] = negt
        fsc = np.zeros((P, 18), np.float32)
        fsc[:, 0:16] = 2.0 / (2 * TL)
        fsc[0, 0] = 1.0 / (2 * TL)
        fsc[:, 16:18] = 2.0 / (2 * TC)
        fsc[0, 16] = 1.0 / (2 * TC)
        _CONST_CACHE["hy_fsc"] = fsc
        deltas = np.abs(np.linspace(HY_MIN_DECAY, HY_MAX_DECAY, 512, dtype=np.float32))
        _CONST_CACHE["hy_delt"] = np.ascontiguousarray(np.broadcast_to(deltas[None, :], (P, 512))).astype(np.float32)
        alt = ((-1.0) ** np.arange(TL)).astype(np.float32)
        _CONST_CACHE["hy_altc"] = alt[:P].reshape(P, 1).astype(ml_dtypes.bfloat16)
        _CONST_CACHE["hy_altr"] = alt.reshape(1, TL).astype(ml_dtypes.bfloat16)
        c, s_ = make_dft(TL)
        _CONST_CACHE["dftC_lat"] = c
        _CONST_CACHE["dftS_lat"] = s_
        c, s_ = make_dft(TC)
        _CONST_CACHE["dftC_ctx"] = c
        _CONST_CACHE["dftS_ctx"] = s_
    return _CONST_CACHE


def prep_shared(inputs):
    g = {k: np.asarray(v) for k, v in inputs.items()}
    vec = np.zeros((P, NV), np.float32)

    def put(name, i, v):
        c = _cols(v)
        o = VEC_OFF[name] + i
        vec[:, o:o + c.shape[1]] = c
    for l in range(2):
        put("ada_b", l * 48, g["ada_b"][l])
        for w in range(2):
            put("ln_g", (l * 2 + w) * 8, g["ln_g"][l, w])
            put("ln_b", (l * 2 + w) * 8, g["ln_b"][l, w])
    for tp in range(4):
        put("dn_conv_w", tp * 12, g["dn_conv_w"][0, tp])
        put("lru_conv_w", tp * 8, g["lru_conv_w"][0, tp])
    for tp in range(3):
        put("hy_conv_w", tp * 12, g["hy_conv_w"][0, tp])
    put("hy_conv_b", 0, g["hy_conv_b"][0])
    put("hy_skip", 0, g["hy_skip"][0])
    put("od_b_in", 0, g["od_b_in"][0])
    put("lru_conv_b", 0, g["lru_conv_b"][0])
    for d in range(2):
        put("lru_ba", d * 8, g["lru_ba"][0, d])
        put("lru_bx", d * 8, g["lru_bx"][0, d])
        put("lru_a", d * 8, g["lru_a_param"][0, d])
    put("od_b_out", 0, g["od_b_out"][0])
    put("dn_norm_g", 0, g["dn_norm_g"][0])
    o = VEC_OFF["hy_mlp"]
    vec[:64, o + 0] = g["hy_b_in"][0]
    vec[:64, o + 1] = g["hy_b_mid"][0, 0]
    vec[:64, o + 2] = g["hy_b_mid"][0, 1]
    vec[:64, o + 3] = g["hy_freq"][0]
    b1 = g["moe_b1"]
    b1d = np.concatenate([b1[..., 0::2], b1[..., 1::2]], axis=-1)
    o = VEC_OFF["moe_b1"]
    vec[:, o:o + 1024] = b1d.reshape(2 * 32 * 16, P).T
    gparams = np.zeros((16, 2), np.float32)
    for d in range(2):
        gparams[d * 8:d * 8 + 4, 0] = g["dn_dt_bias"][0, d]
        gparams[d * 8:d * 8 + 4, 1] = g["dn_a_log"][0, d]
    sh = {"vec": vec, "ada_w": g["ada_w"], "ada_b_raw": g["ada_b"], "ev_w_in": g["ev_w_in"][0], "gparams": gparams,
          "hy_w_in": g["hy_w_in"][0], "hy_w_mid": g["hy_w_mid"][0], "hy_w_out": g["hy_w_out"][0],
          "ev_w_out": g["ev_w_out"][0], "od_w_out": g["od_w_out"][0], "router_w": g["router_w"], "router_b": g["router_b"],
          "od_w_in": g["od_w_in"][0], "lru_wa": g["lru_wa"][0], "lru_wx": g["lru_wx"][0]}
    if "moe_w1" in g:
        w1 = g["moe_w1"].reshape(2, NE, 8, P, 2, 4, P, 2)
        sh["moe_w1r"] = np.ascontiguousarray(w1.transpose(0, 1, 4, 3, 2, 5, 7, 6)).reshape(2, NE, 2, P, 8, 1024)
        w2 = g["moe_w2"].reshape(2, NE, 8, P, D)
        sh["moe_w2r"] = np.ascontiguousarray(w2.transpose(0, 1, 3, 2, 4))
        sh["moe_b2"] = g["moe_b2"]
    sh.update(host_consts())
    return sh


def prep_core(inputs, b):
    x = np.asarray(inputs["x"][b])
    ctx = np.asarray(inputs["ctx"][b])
    xT0 = np.ascontiguousarray(np.concatenate([x, ctx], axis=0).T)
    cc = np.stack([np.asarray(inputs["c"][b]), np.asarray(inputs["c_ctx"])], axis=-1)
    cc = np.ascontiguousarray(cc.reshape(8, P, 2).transpose(1, 0, 2).reshape(P, 16))
    return {"xT0": xT0, "cc": cc}


FULL_PHASES = ["mod", "l0_proj", "dn_prep", "dn_core", "dn_post", "hy_prep", "hy_main", ("post", 0), ("moe", 0),
               "l1_proj", "lru", ("post", 1), ("moe", 1)]


def kernel(**inputs):
    cfg = {"phases": FULL_PHASES, "dump": []}
    nc, K = build_program(cfg)
    sh = prep_shared(inputs)
    in_names = [n for n in K.d if n in sh or n in ("xT0", "cc")]
    maps = []
    for b in range(8):
        m = dict(sh)
        m.update(prep_core(inputs, b))
        maps.append({k: m[k] for k in in_names})
    res = run_bass_kernel_spmd(nc, maps, core_ids=list(range(8)))
    out = np.stack([np.ascontiguousarray(np.asarray(r["out"]).T) for r in res.results], axis=0)
    return out.astype(np.float32)
```
